# Optimizing a Trainium2 kernel written in Bass

```python
import math
import jax, jax.numpy as jnp
from jax import lax
import numpy as np

D_MODEL = 1024
BATCH = 2
SEQ = 8192
DEPTH = 1

HEAD_DIM = 64
N_HEADS_MOBA = 8
N_HEADS_SB = 8
W_MOBA = N_HEADS_MOBA * HEAD_DIM
W_SB = N_HEADS_SB * HEAD_DIM
MOBA_BLOCK = 256
MOBA_TOPK = 3
Q_CHUNK = 128
PEER_HEADS = 8
PEER_NKEYS = 128
PEER_N_EXPERTS = PEER_NKEYS * PEER_NKEYS
PEER_QDIM = 256
PEER_HALF = PEER_QDIM // 2
PEER_TOPK = 16
PEER_TOK_CHUNK = 128
RMS_EPS = 1e-6
IN_SPLITS = [W_MOBA, W_MOBA, W_MOBA, W_SB, W_SB, W_SB, D_MODEL, D_MODEL]
IN_WIDTH = sum(IN_SPLITS)

kernel_name = "hybrid_moba_stickbreak_peer_block"


def rmsnorm(x, g):
    xf = x.astype(jnp.float32)
    y = xf * lax.rsqrt(jnp.mean(xf * xf, axis=-1, keepdims=True) + RMS_EPS)
    return (y * g.astype(jnp.float32)).astype(x.dtype)


def alibi_slopes(n_heads):
    return jnp.exp2(-8.0 * jnp.arange(1, n_heads + 1, dtype=jnp.float32) / n_heads)


def _heads(t, n):
    b, s, _ = t.shape
    return t.reshape(b, s, n, HEAD_DIM).transpose(0, 2, 1, 3)


def _merge(t):
    b, h, s, dh = t.shape
    return t.transpose(0, 2, 1, 3).reshape(b, s, h * dh)


def moba_attention(q, k, v):
    B, H, S, dh = q.shape
    nb = -(-S // MOBA_BLOCK)
    pad = nb * MOBA_BLOCK - S
    kp = jnp.pad(k, ((0, 0), (0, 0), (0, pad), (0, 0)))
    vp = jnp.pad(v, ((0, 0), (0, 0), (0, pad), (0, 0)))
    kb = kp.reshape(B, H, nb, MOBA_BLOCK, dh)
    vb = vp.reshape(B, H, nb, MOBA_BLOCK, dh)
    kmean = jnp.mean(kb.astype(jnp.float32), axis=3)
    ksel = min(MOBA_TOPK, nb)
    scale = dh ** -0.5
    slope = alibi_slopes(H).reshape(1, H, 1, 1)
    slope5 = slope[..., None]
    bidx = jnp.arange(B)[:, None, None, None]
    hidx = jnp.arange(H)[None, :, None, None]
    n_chunks = S // Q_CHUNK
    qc = q.reshape(B, H, n_chunks, Q_CHUNK, dh).transpose(2, 0, 1, 3, 4)

    def chunk(args):
        q_blk, c = args
        qf = q_blk.astype(jnp.float32)
        t = c * Q_CHUNK + jnp.arange(Q_CHUNK)
        own = (c * Q_CHUNK) // MOBA_BLOCK
        bs = jnp.einsum('bhqd,bhnd->bhqn', qf, kmean)
        bs = jnp.where(jnp.arange(nb) < own, bs, -jnp.inf)
        _, top_i = lax.top_k(bs, ksel)
        valid = top_i < own
        k_sel = kb[bidx, hidx, top_i].astype(jnp.float32)
        v_sel = vb[bidx, hidx, top_i].astype(jnp.float32)
        s_pos = top_i[..., None] * MOBA_BLOCK + jnp.arange(MOBA_BLOCK)
        dist_sel = (t[:, None, None] - s_pos).astype(jnp.float32)
        sc_sel = jnp.einsum('bhqd,bhqnkd->bhqnk', qf, k_sel) * scale - slope5 * dist_sel
        sc_sel = jnp.where(valid[..., None], sc_sel, -jnp.inf).reshape(B, H, Q_CHUNK, ksel * MOBA_BLOCK)
        k_own = lax.dynamic_slice_in_dim(kp, own * MOBA_BLOCK, MOBA_BLOCK, axis=2).astype(jnp.float32)
        v_own = lax.dynamic_slice_in_dim(vp, own * MOBA_BLOCK, MOBA_BLOCK, axis=2).astype(jnp.float32)
        s_own = own * MOBA_BLOCK + jnp.arange(MOBA_BLOCK)
        dist_own = (t[:, None] - s_own[None, :]).astype(jnp.float32)
        sc_own = jnp.einsum('bhqd,bhkd->bhqk', qf, k_own) * scale - slope * dist_own
        sc_own = jnp.where(s_own[None, :] <= t[:, None], sc_own, -jnp.inf)
        p = jax.nn.softmax(jnp.concatenate([sc_sel, sc_own], axis=-1), axis=-1)
        p_sel = p[..., :ksel * MOBA_BLOCK].reshape(B, H, Q_CHUNK, ksel, MOBA_BLOCK)
        p_own = p[..., ksel * MOBA_BLOCK:]
        o = (jnp.einsum('bhqnk,bhqnkd->bhqd', p_sel, v_sel)
             + jnp.einsum('bhqk,bhkd->bhqd', p_own, v_own))
        return o.astype(q.dtype)

    out = lax.map(chunk, (qc, jnp.arange(n_chunks)))
    return out.transpose(1, 2, 0, 3, 4).reshape(B, H, S, dh)


def stick_breaking_attention(q, k, v):
    B, H, S, dh = q.shape
    scale = dh ** -0.5
    kf = k.astype(jnp.float32)
    vf = v.astype(jnp.float32)
    s_idx = jnp.arange(S)
    n_chunks = S // Q_CHUNK
    qc = q.reshape(B, H, n_chunks, Q_CHUNK, dh).transpose(2, 0, 1, 3, 4)

    def chunk(args):
        q_blk, c = args
        t = c * Q_CHUNK + jnp.arange(Q_CHUNK)
        z = jnp.einsum('bhqd,bhkd->bhqk', q_blk.astype(jnp.float32), kf) * scale
        strict = s_idx[None, :] < t[:, None]
        log_1m = jnp.where(strict, jax.nn.log_sigmoid(-z), 0.0)
        suffix = lax.cumsum(log_1m, axis=3, reverse=True) - log_1m
        w = jnp.where(strict, jnp.exp(jax.nn.log_sigmoid(z) + suffix), 0.0)
        return jnp.einsum('bhqk,bhkd->bhqd', w, vf).astype(q.dtype)

    out = lax.map(chunk, (qc, jnp.arange(n_chunks)))
    return out.transpose(1, 2, 0, 3, 4).reshape(B, H, S, dh)


def peer_ffn(x, w_q, sub_keys, expert_u, expert_v):
    B, S, D = x.shape
    T = B * S
    xt = x.reshape(T // PEER_TOK_CHUNK, PEER_TOK_CHUNK, D)
    skf = sub_keys.astype(jnp.float32)

    def chunk(x_blk):
        tc = x_blk.shape[0]
        qh = (x_blk @ w_q).astype(jnp.float32).reshape(tc, PEER_HEADS, 2, PEER_HALF)
        scores = jnp.einsum('thpc,hpnc->thpn', qh, skf)
        s_top, i_top = lax.top_k(scores, PEER_TOPK)
        cand = s_top[:, :, 0, :, None] + s_top[:, :, 1, None, :]
        cand_idx = i_top[:, :, 0, :, None] * PEER_NKEYS + i_top[:, :, 1, None, :]
        c_s, c_pos = lax.top_k(cand.reshape(tc, PEER_HEADS, PEER_TOPK * PEER_TOPK), PEER_TOPK)
        e_idx = jnp.take_along_axis(cand_idx.reshape(tc, PEER_HEADS, PEER_TOPK * PEER_TOPK), c_pos, axis=-1)
        g = jax.nn.softmax(c_s, axis=-1)
        u = expert_u[e_idx].astype(jnp.float32)
        v = expert_v[e_idx].astype(jnp.float32)
        a = jax.nn.gelu(jnp.einsum('td,thkd->thk', x_blk.astype(jnp.float32), u), approximate=False)
        return jnp.einsum('thk,thkd->td', g * a, v).astype(x.dtype)

    return lax.map(chunk, xt).reshape(B, S, D)


def setup_inputs(seed: int = 0) -> dict:
    key = jax.random.key(seed)
    ks = jax.random.split(key, 12)
    f32 = jnp.float32
    x = jax.random.normal(ks[0], (BATCH, SEQ, D_MODEL), f32)
    norm1_g = 1.0 + 0.01 * jax.random.normal(ks[1], (DEPTH, D_MODEL), f32)
    w_in = jax.random.normal(ks[2], (DEPTH, D_MODEL, IN_WIDTH), f32) * D_MODEL ** -0.5
    w_out_moba = jax.random.normal(ks[3], (DEPTH, W_MOBA, D_MODEL), f32) * W_MOBA ** -0.5
    w_out_sb = jax.random.normal(ks[4], (DEPTH, W_SB, D_MODEL), f32) * W_SB ** -0.5
    w_mix_out = jax.random.normal(ks[5], (DEPTH, D_MODEL, D_MODEL), f32) * D_MODEL ** -0.5
    norm2_g = 1.0 + 0.01 * jax.random.normal(ks[6], (DEPTH, D_MODEL), f32)
    peer_w_q = jax.random.normal(ks[7], (DEPTH, D_MODEL, PEER_HEADS * PEER_QDIM), f32) * D_MODEL ** -0.5
    peer_sub_keys = jax.random.normal(ks[8], (DEPTH, PEER_HEADS, 2, PEER_NKEYS, PEER_HALF), f32) * PEER_HALF ** -0.5
    peer_u = jax.random.normal(ks[9], (DEPTH, PEER_N_EXPERTS, D_MODEL), f32) * D_MODEL ** -0.5
    peer_v = jax.random.normal(ks[10], (DEPTH, PEER_N_EXPERTS, D_MODEL), f32) * PEER_HEADS ** -0.5
    final_norm_g = 1.0 + 0.01 * jax.random.normal(ks[11], (D_MODEL,), f32)
    return {"x": x, "norm1_g": norm1_g, "w_in": w_in, "w_out_moba": w_out_moba,
            "w_out_sb": w_out_sb, "w_mix_out": w_mix_out, "norm2_g": norm2_g,
            "peer_w_q": peer_w_q, "peer_sub_keys": peer_sub_keys, "peer_u": peer_u,
            "peer_v": peer_v, "final_norm_g": final_norm_g}


def reference(x, norm1_g, w_in, w_out_moba, w_out_sb, w_mix_out, norm2_g,
              peer_w_q, peer_sub_keys, peer_u, peer_v, final_norm_g):
    split_at = [int(i) for i in np.cumsum(IN_SPLITS)[:-1]]
    h = x
    for l in range(DEPTH):
        xn = rmsnorm(h, norm1_g[l])
        proj = xn @ w_in[l]
        q_a, k_a, v_a, q_b, k_b, v_b, gate_a, gate_b = jnp.split(proj, split_at, axis=-1)
        y_a = _merge(moba_attention(_heads(q_a, N_HEADS_MOBA), _heads(k_a, N_HEADS_MOBA),
                                    _heads(v_a, N_HEADS_MOBA))) @ w_out_moba[l]
        y_b = _merge(stick_breaking_attention(_heads(q_b, N_HEADS_SB), _heads(k_b, N_HEADS_SB),
                                              _heads(v_b, N_HEADS_SB))) @ w_out_sb[l]
        mixed = jax.nn.sigmoid(gate_a) * y_a + jax.nn.sigmoid(gate_b) * y_b
        h = h + mixed @ w_mix_out[l]
        h = h + peer_ffn(rmsnorm(h, norm2_g[l]), peer_w_q[l], peer_sub_keys[l], peer_u[l], peer_v[l])
    return rmsnorm(h, final_norm_g)
```

```python
import contextlib
import numpy as np
import concourse.bass as bass
import concourse.mybir as mybir
from concourse.bass_utils import run_bass_kernel_spmd

F32 = mybir.dt.float32
BF16 = mybir.dt.bfloat16
U32 = mybir.dt.uint32
I32 = mybir.dt.int32
AF = mybir.ActivationFunctionType
ALU = mybir.AluOpType
AX = mybir.AxisListType

S_LEN = 8192
D = 1024
NT_ALL = 64
NT_OWN = 16
DH = 64
NEG = -240000.0
EPS = 1e-6

ENGINES = ("pe", "act", "dve", "pool", "sp")
N_DMA_SEMS = 48


class V:
    __slots__ = ("ap", "res")

    def __init__(self, ap, res):
        self.ap = ap
        self.res = res if isinstance(res, tuple) else (res,)


class T:
    def __init__(self, t, name):
        self.t = t
        self.name = name

    def __getitem__(self, idx):
        return V(self.t[idx], self.name)

    def v(self, ap, sub=None):
        return V(ap, self.name if sub is None else self.name + "/" + sub)


class Op:
    __slots__ = ("eng", "fn", "deps", "idx", "is_dma", "signal", "count", "sem")


def _res(xs):
    out = []
    for x in xs:
        if x is None:
            continue
        if isinstance(x, V):
            out.extend(x.res)
        elif isinstance(x, str):
            out.append(x)
        elif isinstance(x, (int, float)):
            continue
        else:
            out.extend(x)
    return out


class Sched:
    def __init__(self, nc, same_engine_sync=True):
        self.nc = nc
        self.q = {e: [] for e in ENGINES}
        self.last_w = {}
        self.readers = {}
        self.same_engine_sync = same_engine_sync
        self.n_dma = 0
        self.dma_ops = []
        self.sw_dma_ops = []

    def op(self, eng, fn, reads=(), writes=(), dma=False, extra_deps=()):
        o = Op()
        o.eng = eng
        o.fn = fn
        o.is_dma = dma
        o.signal = False
        o.count = None
        o.sem = None
        o.idx = -1
        deps = []
        rres = _res(reads)
        wres = _res(writes)
        for r in rres:
            lw = self.last_w.get(r)
            if lw is not None:
                deps.append(lw)
        for w in wres:
            lw = self.last_w.get(w)
            if lw is not None:
                deps.append(lw)
            deps.extend(self.readers.get(w, ()))
        deps.extend(extra_deps)
        seen = set()
        fdeps = []
        for d in deps:
            if id(d) in seen or d is o:
                continue
            seen.add(id(d))
            if (not d.is_dma) and d.eng == eng:
                if eng == "pe" or not self.same_engine_sync:
                    continue
            fdeps.append(d)
        o.deps = fdeps
        for d in fdeps:
            d.signal = True
        for w in wres:
            self.last_w[w] = o
            self.readers[w] = []
        for r in rres:
            self.readers.setdefault(r, []).append(o)
        if dma and eng == "pool":
            o.signal = True
            o.idx = -2
            self.sw_dma_ops.append(o)
        elif dma:
            o.signal = True
            o.idx = self.n_dma
            self.n_dma += 1
            self.dma_ops.append(o)
        self.q[eng].append(o)
        return o

    def barrier(self):
        lasts = []
        for e in ENGINES:
            for o in reversed(self.q[e]):
                if not o.is_dma:
                    lasts.append(o)
                    break
        lasts.extend(self.dma_ops[-N_DMA_SEMS:])
        lasts.extend(self.sw_dma_ops)
        for e in ENGINES:
            self.op(e, lambda eng: eng.nop(), extra_deps=lasts)
        self.last_w = {}
        self.readers = {}

    def mm(self, out, lhsT, rhs, start=True, stop=True):
        return self.op("pe", lambda e: e.matmul(out.ap, lhsT.ap, rhs.ap, start=start, stop=stop),
                       reads=[lhsT, rhs] + ([] if start else [out]), writes=[out])

    def tr(self, out, in_, ident):
        return self.op("pe", lambda e: e.transpose(out.ap, in_.ap, ident.ap),
                       reads=[in_, ident], writes=[out])

    def act(self, out, in_, func, bias=0.0, scale=1.0, accum=None, eng="act"):
        b = bias.ap if isinstance(bias, V) else float(bias)
        s = scale.ap if isinstance(scale, V) else float(scale)
        kw = {}
        if accum is not None:
            kw["accum_out"] = accum.ap
        return self.op(eng, lambda e: e.activation(out.ap, in_.ap, func, bias=b, scale=s, **kw),
                       reads=[in_, bias, scale], writes=[out, accum])

    def tt(self, out, in0, in1, op, eng="dve"):
        return self.op(eng, lambda e: e.tensor_tensor(out.ap, in0.ap, in1.ap, op),
                       reads=[in0, in1], writes=[out])

    def ts(self, out, in0, s1, op0, s2=None, op1=None, eng="dve", accum=None):
        a1 = s1.ap if isinstance(s1, V) else s1
        a2 = s2.ap if isinstance(s2, V) else s2
        kw = {}
        if op1 is not None:
            kw["op1"] = op1
        if accum is not None:
            kw["accum_out"] = accum.ap
        return self.op(eng, lambda e: e.tensor_scalar(out.ap, in0.ap, a1, a2, op0, **kw),
                       reads=[in0, s1, s2], writes=[out, accum])

    def stt(self, out, in0, scalar, in1, op0, op1):
        a = scalar.ap if isinstance(scalar, V) else scalar
        return self.op("dve", lambda e: e.scalar_tensor_tensor(out.ap, in0.ap, a, in1.ap, op0, op1),
                       reads=[in0, scalar, in1], writes=[out])

    def copy(self, out, in_, eng="dve"):
        if eng == "act":
            return self.op("act", lambda e: e.copy(out.ap, in_.ap), reads=[in_], writes=[out])
        return self.op(eng, lambda e: e.tensor_copy(out.ap, in_.ap), reads=[in_], writes=[out])

    def red(self, out, in_, op, axis=AX.X, eng="dve"):
        return self.op(eng, lambda e: e.tensor_reduce(out.ap, in_.ap, axis, op),
                       reads=[in_], writes=[out])

    def memset(self, out, val, eng="dve"):
        return self.op(eng, lambda e: e.memset(out.ap, val), writes=[out])

    def recip(self, out, in_):
        return self.op("dve", lambda e: e.reciprocal(out.ap, in_.ap), reads=[in_], writes=[out])

    def dma(self, out, in_, eng="sp", **kw):
        return self.op(eng, lambda e: e.dma_start(out.ap, in_.ap, **kw),
                       reads=[in_], writes=[out], dma=True)

    def emit(self, stack):
        nc = self.nc
        sems = {e: stack.enter_context(nc.semaphore("s_" + e)) for e in ENGINES}
        dsems = [stack.enter_context(nc.semaphore("d%d" % i)) for i in range(N_DMA_SEMS)]
        swsems = [stack.enter_context(nc.semaphore("w%d" % i)) for i in range(len(self.sw_dma_ops))]
        for i, o in enumerate(self.sw_dma_ops):
            o.sem = swsems[i]
            o.count = 16
        for e in ENGINES:
            c = 0
            for o in self.q[e]:
                if o.is_dma and o.idx == -2:
                    continue
                if o.is_dma:
                    o.sem = dsems[o.idx % N_DMA_SEMS]
                    o.count = 16 * (o.idx // N_DMA_SEMS + 1)
                elif o.signal:
                    c += 1
                    o.sem = sems[e]
                    o.count = c
        final_waits = list(self.dma_ops[-N_DMA_SEMS:]) + list(self.sw_dma_ops)
        block = stack.enter_context(nc.Block())
        engmap = {"pe": block.tensor, "act": block.scalar, "dve": block.vector,
                  "pool": block.gpsimd, "sp": block.sync}
        for e in ENGINES:
            ops = self.q[e]

            def body(eng, ops=ops, e=e):
                waited = {}

                def wait(d):
                    key = id(d.sem)
                    if waited.get(key, 0) >= d.count:
                        return
                    eng.wait_ge(d.sem, d.count)
                    waited[key] = d.count

                for o in ops:
                    if o.is_dma and o.idx >= N_DMA_SEMS:
                        wait(self.dma_ops[o.idx - N_DMA_SEMS])
                    for d in o.deps:
                        wait(d)
                    ins = o.fn(eng)
                    if o.signal:
                        ins.then_inc(o.sem, 16 if o.is_dma else 1)
                if e == "sp":
                    for d in final_waits:
                        wait(d)

            engmap[e](body)


SAME_ENGINE_SYNC = True
SBUF_BASE = 16384
SBUF_TOP = 192 * 1024


def skew(S, items, stages):
    n, ns = len(items), len(stages)
    for step in range(n + ns - 1):
        for st in range(ns):
            i = step - st
            if 0 <= i < n and stages[st] is not None:
                stages[st](items[i], i)


class Builder:
    def __init__(self, ns=NT_OWN, dbg=(), upto=99, nexp=128):
        self.ns = ns
        self.nkt = min(NT_ALL, 4 * ns)
        self.upto = upto
        self.dbg = set(dbg)
        self.nc = bass.Bass("TRN2", target_bir_lowering=False)
        self.S = Sched(self.nc, same_engine_sync=SAME_ENGINE_SYNC)
        self.top = SBUF_BASE
        self.nexp_chunks = nexp
        self.uid = 0

    def sb(self, name, shape, dt=F32):
        esz = 2 if dt == BF16 else 4
        n = 1
        for x in shape[1:]:
            n *= x
        nbytes = (n * esz + 63) // 64 * 64
        off = self.top
        self.top += nbytes
        self.hw = max(getattr(self, 'hw', 0), self.top)
        assert self.top <= SBUF_TOP, ("SBUF overflow", name, self.top)
        self.uid += 1
        nm = "%s_%d" % (name, self.uid)
        t = self.nc.alloc_sbuf_tensor_at(nm, list(shape), dt, offset=off)
        return T(t, nm)

    def mark(self):
        return self.top

    def release(self, m):
        self.S.barrier()
        self.top = m

    def dram(self, name, shape, dt, kind="Internal"):
        if name in self.dbg:
            kind = "ExternalOutput"
        t = self.nc.dram_tensor(name, list(shape), dt, kind=kind)
        return T(t.ap(), name)

    def bank(self, i, shape=None, dt=F32):
        return self.banks[i]

    def build(self):
        S = self.S
        stack = contextlib.ExitStack()
        with stack:
            self.banks = []
            self.banks_bf = []
            for i in range(8):
                t = self.nc.alloc_psum_tensor("bank%d" % i, [128, 512], F32)
                self.banks.append(T(t, "bank%d" % i))
                self.banks_bf.append(T(t[:].bitcast(BF16), "bank%d" % i))
            self.declare_io()
            self.consts()
            if self.upto >= 1:
                self.phase_q()
            if self.upto >= 2:
                self.phase_kv()
            if self.upto >= 3:
                self.phase_attn()
            if self.upto >= 5:
                self.xn2T = self.sb("xn2T", [128, 8, self.ns * 128], BF16)
                self.g2 = self.sb("g2", [128, 8], F32)
                S.dma(self.g2[:], V(self.norm2_g.t.rearrange("(c p) -> p c", p=128), "norm2_g"),
                      allow_slow_non_contiguous=True)
                self.phase_mix()
            if self.upto >= 6:
                self.phase_peer()
            S.emit(stack)
        return self.nc

    def declare_io(self):
        d = self.dram
        EI = "ExternalInput"
        self.xall = d("xall", [S_LEN, D], F32, EI)
        self.xown = d("xown", [NT_OWN * 128, D], F32, EI)
        self.norm1_g = d("norm1_g", [D], F32, EI)
        self.w_in = d("w_in", [D, 5120], F32, EI)
        self.sbm01 = d("sbm01", [128, 512], F32, EI)
        self.mbneg = d("mbneg", [128, 512], F32, EI)
        self.kcst = d("kcst", [128, S_LEN], F32, EI)
        self.cvec = d("cvec", [128, 16], F32, EI)
        self.w_out_moba = d("w_out_moba", [512, D], F32, EI)
        self.w_out_sb = d("w_out_sb", [512, D], F32, EI)
        self.w_mix_out = d("w_mix_out", [D, D], F32, EI)
        self.norm2_g = d("norm2_g", [D], F32, EI)
        self.peer_w_q = d("peer_w_q", [D, 2048], F32, EI)
        self.peer_sub_keys = d("peer_sub_keys", [16, 128, 128], F32, EI)
        self.peer_u = d("peer_u", [16384, D], F32, EI)
        self.peer_v = d("peer_v", [16384, D], F32, EI)
        self.final_norm_g = d("final_norm_g", [D], F32, EI)
        self.out = d("out", [NT_OWN * 128, D], F32, "ExternalOutput")
        self.H1 = d("H1", [NT_OWN * 128, D], F32)
        self.UT = d("UT", [128, 128, 8, 128], BF16)
        self.VB = d("VB", [16384, D], BF16)
        self.KT = d("KT", [16, DH, S_LEN], BF16)
        self.VS = d("VS", [S_LEN, 16 * DH], BF16)
        self.KM = d("KM", [128, 4, 32], F32)
        self.QT = d("QT", [16, DH, NT_OWN * 128], BF16)
        self.SG = d("SG", [NT_OWN * 128, 2048], F32)
        self.AT = d("AT", [128, 8, NT_OWN * 128], BF16)

    def consts(self):
        S = self.S
        self.ident_bf = self.sb("ident_bf", [128, 128], BF16)
        self.ident_f = self.sb("ident_f", [128, 128], F32)
        self.iota_free = self.sb("iota_free", [128, 128], F32)
        self.iota_part = self.sb("iota_part", [128, 1], F32)
        self.eps_t = self.sb("eps_t", [128, 1], F32)
        self.one_t = self.sb("one_t", [128, 1], F32)
        S.op("pool", lambda e: e.iota(self.iota_free.t[:], [[1, 128]], base=0, channel_multiplier=0,
                                      allow_small_or_imprecise_dtypes=True), writes=[self.iota_free[:]])
        S.op("pool", lambda e: e.iota(self.iota_part.t[:], [[0, 1]], base=0, channel_multiplier=1,
                                      allow_small_or_imprecise_dtypes=True), writes=[self.iota_part[:]])
        S.ts(self.ident_f[:], self.iota_free[:], self.iota_part[:], ALU.is_equal)
        S.copy(self.ident_bf[:], self.ident_f[:])
        S.memset(self.eps_t[:], EPS)
        S.memset(self.one_t[:], 1.0)
        self.tri_bf = self.sb("tri_bf", [128, 128], BF16)
        self.ones_bf = self.sb("ones_bf", [128, 128], BF16)
        S.ts(self.tri_bf[:], self.iota_free[:], self.iota_part[:], ALU.is_le)
        S.memset(self.ones_bf[:], 1.0)

    def rms_tile(self, xt, xn_bf, ss, rstd, junk, gB=None):
        S = self.S
        S.act(junk[:], xt[:], AF.Square, accum=ss[:])
        S.act(rstd[:], ss[:], AF.Ln, bias=self.eps_t[:], scale=1.0 / D)
        S.act(rstd[:], rstd[:], AF.Exp, scale=-0.5)
        if gB is None:
            S.ts(xn_bf[:], xt[:], rstd[:], ALU.mult)
        else:
            S.stt(xn_bf[:], xt[:], rstd[:], gB[:], ALU.mult, ALU.mult)

    def load_weights_cast(self, dst, src3, col_ranges):
        S = self.S
        o = 0
        for (c0, nc_) in col_ranges:
            S.dma(dst[:, :, o:o + nc_], V(src3[:, :, c0:c0 + nc_], "w_dram"), eng="pool")
            o += nc_

    def phase_q(self):
        S = self.S
        g1B = self.g1B = self.sb("g1B", [128, D], F32)
        S.dma(g1B[:], V(self.norm1_g.t.partition_broadcast(128), "norm1_g"), eng="sp")
        w3 = self.w_in.t.rearrange("(c p) n -> p c n", p=128)
        self.mk_wkv = self.mark()
        self.wkv = self.sb("wkv", [128, 8, 2048], BF16)
        mk2 = self.mark()
        wq = self.sb("wq", [128, 8, 1024], BF16)
        wg = self.sb("wg", [128, 8, 2048], BF16)
        self.load_weights_cast(wq, w3, [(0, 512), (1536, 512)])
        self.load_weights_cast(wg, w3, [(3072, 2048)])
        self.load_weights_cast(self.wkv, w3, [(512, 512), (2048, 512), (1024, 512), (2560, 512)])
        xt = [self.sb("xt%d" % i, [128, D], F32) for i in range(3)]
        xn = [self.sb("xn%d" % i, [128, D], BF16) for i in range(2)]
        junk = self.sb("junk", [128, D], BF16)
        ss = [self.sb("ss%d" % i, [128, 1], F32) for i in range(2)]
        rstd = [self.sb("rstd%d" % i, [128, 1], F32) for i in range(2)]
        xnT = [self.sb("xnT%d" % i, [128, 8, 128], BF16) for i in range(2)]
        qsb = [self.sb("qsb%d" % i, [128, 8, 128], BF16) for i in range(2)]
        sg = [self.sb("sg%d" % i, [128, 2048], F32) for i in range(2)]
        xo = self.xown.t.rearrange("(t p) d -> t p d", p=128)
        qt3 = self.QT.t.rearrange("(cc two) d t -> (two d) cc t", two=2)

        def s0(m, i):
            S.dma(xt[m % 3][:], V(xo[m], "xown"), eng="sp")

        def s1(m, i):
            b = m % 2
            self.rms_tile(xt[m % 3], xn[b], ss[b], rstd[b], junk, gB=self.g1B)

        def s2(m, i):
            b = m % 2
            pTb = self.banks_bf[b]
            for c in range(8):
                S.tr(pTb[:, c * 128:(c + 1) * 128], xn[b][:, c * 128:(c + 1) * 128], self.ident_bf[:])
            S.copy(xnT[b][:], pTb.v(pTb.t[:, 0:1024].rearrange("p (c t) -> p c t", t=128)), eng="dve")

        def s3(m, i):
            b = m % 2
            for half in range(2):
                pq = self.banks[2 + half]
                for k in range(4):
                    cc = half * 4 + k
                    for c in range(8):
                        S.mm(pq[:, k * 128:(k + 1) * 128], wq[:, c, cc * 128:(cc + 1) * 128], xnT[b][:, c, :],
                             start=(c == 0), stop=(c == 7))
                S.copy(qsb[b][:, half * 4:(half + 1) * 4, :],
                       pq.v(pq.t[:].rearrange("p (k t) -> p k t", t=128)), eng="act")
            S.dma(V(qt3[:, :, m * 128:(m + 1) * 128], "QT"), qsb[b][:], eng="act")
            for k in range(4):
                pg = self.banks[4 + k]
                for c in range(8):
                    S.mm(pg[:], xnT[b][:, c, :], wg[:, c, k * 512:(k + 1) * 512], start=(c == 0), stop=(c == 7))
                S.act(sg[b][:, k * 512:(k + 1) * 512], pg[:], AF.Exp, scale=-1.0)
            S.ts(sg[b][:], sg[b][:], 1.0, ALU.add)
            S.recip(sg[b][:], sg[b][:])
            S.dma(self.SG[m * 128:(m + 1) * 128, :], sg[b][:], eng="act")

        skew(S, list(range(self.ns)), [s0, None, s1, s2, s3])
        self.release(mk2)

    def phase_kv(self):
        S = self.S
        mk = self.mark()
        wkv = self.wkv
        xt = [self.sb("xt%d" % i, [128, D], F32) for i in range(3)]
        xn = [self.sb("xn%d" % i, [128, D], BF16) for i in range(2)]
        junk = self.sb("junk", [128, D], BF16)
        ss = [self.sb("ss%d" % i, [128, 1], F32) for i in range(2)]
        rstd = [self.sb("rstd%d" % i, [128, 1], F32) for i in range(2)]
        xnT = [self.sb("xnT%d" % i, [128, 8, 512], BF16) for i in range(2)]
        ksb = [self.sb("ksb%d" % i, [128, 512], BF16) for i in range(4)]
        vsb = [self.sb("vsb%d" % i, [128, 1024], BF16) for i in range(3)]
        kmT = self.sb("kmT", [128, 4, 32], F32)
        S.memset(kmT[:], 0.0)
        xa = self.xall.t.rearrange("(t p) d -> t p d", p=128)
        kt4 = self.KT.t.rearrange("(cc two) d t -> cc (two d) t", two=2)
        cnt = {"pk": 0, "ks": 0, "conv": 0}
        pK = [self.banks[2 + i] for i in range(4)]
        nexp = self.nexp_chunks
        bk = self.banks
        def xsub(cb, j):
            return "xnT%d/t%d" % (cb, j)

        def s0(tt, i):
            S.dma(xt[tt % 3][:], V(xa[tt], "xall"), eng="sp")

        def s1(tt, i):
            b = tt % 2
            self.rms_tile(xt[tt % 3], xn[b], ss[b], rstd[b], junk, gB=self.g1B)

        def s2(tt, i):
            b = tt % 2
            cb = (tt // 4) % 2
            j = tt % 4
            pTb = self.banks_bf[b]
            for c in range(8):
                S.tr(pTb[:, c * 128:(c + 1) * 128], xn[b][:, c * 128:(c + 1) * 128], self.ident_bf[:])
            S.copy(V(xnT[cb].t[:, :, j * 128:(j + 1) * 128], xsub(cb, j)),
                   pTb.v(pTb.t[:, 0:1024].rearrange("p (c t) -> p c t", t=128)), eng="dve")

        def s3(tt, i):
            b = tt % 2
            ch = tt // 4
            cb = ch % 2
            j = tt % 4
            for vh in range(2):
                pk = pK[cnt["pk"] % 4]
                cnt["pk"] += 1
                for c in range(8):
                    S.mm(pk[:], V(xnT[cb].t[:, c, j * 128:(j + 1) * 128], xsub(cb, j)),
                         wkv[:, c, 1024 + vh * 512:1024 + (vh + 1) * 512], start=(c == 0), stop=(c == 7))
                S.copy(vsb[tt % 3][:, vh * 512:(vh + 1) * 512], pk[:], eng="act")
            S.dma(self.VS[tt * 128:(tt + 1) * 128, :], vsb[tt % 3][:], eng="act")
            if j == 3:
                allsub = tuple(xsub(cb, jj) for jj in range(4))
                for cc in range(8):
                    pk = pK[cnt["pk"] % 4]
                    cnt["pk"] += 1
                    for c in range(8):
                        S.mm(pk[:], wkv[:, c, cc * 128:(cc + 1) * 128], V(xnT[cb].t[:, c, :], allsub),
                             start=(c == 0), stop=(c == 7))
                    kb = ksb[cnt["ks"] % 4]
                    cnt["ks"] += 1
                    if cc < 4:
                        for hb in range(2):
                            S.act(kb[:, hb * 256:(hb + 1) * 256], pk[:, hb * 256:(hb + 1) * 256], AF.Copy,
                                  accum=kmT[:, cc, ch * 2 + hb:ch * 2 + hb + 1])
                    else:
                        S.copy(kb[:], pk[:], eng="act")
                    S.dma(V(kt4[cc][:, ch * 512:(ch + 1) * 512], "KT"), kb[:], eng="act")

        skew(S, list(range(self.nkt)), [s0, None, s1, s2, s3])
        S.ts(kmT[:], kmT[:], 1.0 / 256.0, ALU.mult)
        S.dma(self.KM[:], kmT[:], eng="sp")
        self.release(self.mk_wkv)

    def phase_attn(self):
        S = self.S
        mk = self.mark()
        ns, nkt = self.ns, self.nkt
        ncol = ns * 128
        sbm = self.sb("sbm", [128, 512], F32)
        S.dma(sbm[:], self.sbm01[:], eng="sp")
        ktb = [self.sb("ktb%d" % i, [128, nkt * 128], BF16) for i in range(2)]
        vab = [self.sb("vab%d" % i, [128, nkt, DH + 1], BF16) for i in range(2)]
        qtb = [self.sb("qtb%d" % i, [128, ncol], BF16) for i in range(2)]
        for i in range(2):
            S.memset(vab[i][:], 1.0)
            S.memset(V(qtb[i].t[64:128, :], tuple("%s/m%d" % (qtb[i].name, mm_) for mm_ in range(ns))), 0.0)
        cv = self.cv = self.sb("cv", [128, 16], F32)
        S.dma(cv[:], self.cvec[:], eng="sp")
        mbn = self.mbn = self.sb("mbn", [128, 512], F32)
        S.dma(mbn[:], self.mbneg[:], eng="sp")
        self.kl_f = self.sb("kl_f", [128, nkt * 128], F32)
        S.dma(self.kl_f[:], self.kcst[:, 0:nkt * 128], eng="sp")
        for i in range(2):
            S.copy(ktb[i][64:128, :], self.kl_f[64:128, :], eng="dve")
        for r0 in range(0, self.nexp_chunks * 128, 2048):
            r1 = min(self.nexp_chunks * 128, r0 + 2048)
            S.dma(self.VB[r0:r1, :], self.peer_v[r0:r1, :], eng="pool")
        self.kmf = self.sb("kmf", [DH, 32], F32)
        self.kmb = self.sb("kmb", [128, 40], BF16)
        self.bsm = self.sb("bsm", [128, 40], F32)
        self.v8 = self.sb("v8", [128, 8], F32)
        self.thr = self.sb("thr", [128, 1], F32)
        self.selb = self.sb("selb", [128, 34], F32)
        self.offm = self.sb("offm", [128, 34], F32)
        self.MBt = self.sb("MBt", [128, 128], BF16)
        self.den = self.sb("den", [128, 1], F32)
        e_sb = [self.sb("e_sb%d" % i, [128, 512], F32) for i in range(2)]
        sp_bf = [self.sb("sp_bf%d" % i, [128, 512], BF16) for i in range(2)]
        t1 = [self.sb("t1_%d" % i, [128, 512], F32) for i in range(2)]
        w_bf = [self.sb("w_bf%d" % i, [128, 512], BF16) for i in range(3)]
        R_sb = self.sb("R_sb", [128, 128], F32)
        osb = self.sb("osb", [128, ns, 128], BF16)
        otb = [self.sb("otb%d" % i, [128, 128], BF16) for i in range(2)]
        pz = [self.banks[0], self.banks[1], self.banks[2]]
        pC = [self.banks[3], self.banks[7]]
        pR = self.banks[4]
        pO = [self.banks[5], self.banks[6]]
        pTr = V(self.banks_bf[4].t[:, 512:640], "bank4")
        nload = 0
        do_sb = self.upto >= 4 or self.upto == 3
        for mixer in ([1] if self.upto == 3 else ([0] if self.upto == 4 else [0, 1])):
            for hp in range(4):
                for hh in range(2):
                    mh = mixer * 8 + hp * 2 + hh
                    lb = nload % 2
                    nload += 1
                    kt_, va_, qt_ = ktb[lb], vab[lb], qtb[lb]
                    S.dma(kt_[0:DH, :], V(self.KT.t[mh][:, 0:nkt * 128], "KT"), eng="sp")
                    S.dma(va_[:, :, 0:DH], V(self.VS.t[0:nkt * 128, mh * DH:(mh + 1) * DH].rearrange("(t p) d -> p t d", p=128), "VS"), eng="sp")
                    qall = tuple("%s/m%d" % (qt_.name, mm_) for mm_ in range(ns))
                    S.dma(V(qt_.t[0:DH, :], qall), V(self.QT.t[mh][:, 0:ncol], "QT"), eng="sp")
                    if mixer == 1:
                        S.memset(V(qt_.t[64:128, :], qall), 0.0, eng="dve")
                    if mixer == 1:
                        self.sb_head(kt_, va_, qt_, hh, sbm, e_sb, sp_bf, t1, w_bf, R_sb, osb, pz, pC, pR, pO)
                    else:
                        self.moba_head(mh, kt_, va_, qt_, hh, osb, pz, pO, t1, w_bf, pTr)
                for m in range(ns):
                    ob = otb[m % 2]
                    S.tr(pTr, osb[:, m, :], self.ident_bf[:])
                    S.copy(ob[:], pTr, eng="act")
                    S.dma(V(self.AT.t[:, mixer * 4 + hp, m * 128:(m + 1) * 128], "AT"), ob[:], eng="act")
        self.release(mk)

    def phase_mix(self):
        S = self.S
        mk = self.mark()
        woa = self.sb("woa", [128, 4, 1024], BF16)
        wob = self.sb("wob", [128, 4, 1024], BF16)
        wmx = self.sb("wmx", [128, 8, 1024], BF16)
        for (dst, src) in ((woa, self.w_out_moba), (wob, self.w_out_sb), (wmx, self.w_mix_out)):
            self.load_weights_cast(dst, src.t.rearrange("(c p) n -> p c n", p=128), [(0, 1024)])
        at_sb = [self.sb("at_sb%d" % i, [128, 8, 128], BF16) for i in range(2)]
        sg = [self.sb("sgm%d" % i, [128, 2048], F32) for i in range(2)]
        xt = [self.sb("xtm%d" % i, [128, D], F32) for i in range(3)]
        tmp = self.sb("tmpm", [128, D], F32)
        mixed = [self.sb("mixed%d" % i, [128, D], BF16) for i in range(2)]
        mixT = self.sb("mixT", [128, 8, 128], BF16)
        h1 = [self.sb("h1_%d" % i, [128, D], F32) for i in range(2)]
        xn2 = self.sb("xn2", [128, D], BF16)
        junk = self.sb("junkm", [128, D], BF16)
        ss = self.sb("ssm", [128, 1], F32)
        rstd = self.sb("rstdm", [128, 1], F32)
        bk = self.banks

        def m0(m, i):
            b = m % 2
            S.dma(at_sb[b][:], V(self.AT.t[:, :, m * 128:(m + 1) * 128], "AT"), eng="sp")
            S.dma(sg[b][:], self.SG[m * 128:(m + 1) * 128, :], eng="sp")
            S.dma(xt[m % 3][:], self.xown[m * 128:(m + 1) * 128, :], eng="sp")

        def m1(m, i):
            b = m % 2
            for mixer, w in ((0, woa), (1, wob)):
                for hf in range(2):
                    for hp in range(4):
                        S.mm(bk[mixer * 2 + hf][:], at_sb[b][:, mixer * 4 + hp, :], w[:, hp, hf * 512:(hf + 1) * 512],
                             start=(hp == 0), stop=(hp == 3))
            for hf in range(2):
                cs = slice(hf * 512, (hf + 1) * 512)
                S.tt(tmp[:, cs], bk[hf][:], sg[b][:, cs], ALU.mult)
                S.tt(sg[b][:, 1024 + hf * 512:1024 + (hf + 1) * 512], bk[2 + hf][:],
                     sg[b][:, 1024 + hf * 512:1024 + (hf + 1) * 512], ALU.mult)
            S.tt(mixed[b][:], tmp[:], sg[b][:, 1024:2048], ALU.add)

        def m2(m, i):
            b = m % 2
            pT = self.banks_bf[4]
            for c in range(8):
                S.tr(pT[:, c * 128:(c + 1) * 128], mixed[b][:, c * 128:(c + 1) * 128], self.ident_bf[:])
            S.copy(mixT[:], pT.v(pT.t[:, 0:1024].rearrange("p (c t) -> p c t", t=128)), eng="act")
            for hf in range(2):
                for c in range(8):
                    S.mm(bk[5 + hf][:], mixT[:, c, :], wmx[:, c, hf * 512:(hf + 1) * 512], start=(c == 0), stop=(c == 7))
                S.tt(h1[b][:, hf * 512:(hf + 1) * 512], bk[5 + hf][:], xt[m % 3][:, hf * 512:(hf + 1) * 512], ALU.add)

        def m3(m, i):
            b = m % 2
            self.rms_tile(h1[b], xn2, ss, rstd, junk)
            S.dma(self.H1[m * 128:(m + 1) * 128, :], h1[b][:], eng="act")
            pT2 = self.banks_bf[7]
            for c in range(8):
                S.tr(pT2[:, c * 128:(c + 1) * 128], xn2[:, c * 128:(c + 1) * 128], self.ident_bf[:])
            for c in range(8):
                S.act(self.xn2T[:, c, m * 128:(m + 1) * 128], pT2[:, c * 128:(c + 1) * 128], AF.Copy,
                      scale=self.g2[:, c:c + 1])

        skew(S, list(range(self.ns)), [m0, m1, m2, m3])
        self.release(mk)

    def phase_peer(self):
        S = self.S
        ns = self.ns
        bk = self.banks
        ntok = ns * 128
        selT = [self.sb("selT%d" % i, [128, ntok], BF16 if i < 2 else F32) for i in range(3)]
        mkA = self.mark()
        wpq = self.sb("wpq", [128, 8, 2048], BF16)
        w3 = self.peer_w_q.t.rearrange("(c p) n -> p c n", p=128)
        self.load_weights_cast(wpq, w3, [(0, 2048)])
        skT = self.sb("skT", [128, 16, 128], BF16)
        skf = self.sb("skf", [128, 128], F32)
        for hp in range(16):
            S.dma(skf[:], self.peer_sub_keys[hp], eng="sp")
            S.tr(bk[0][:, 0:128], skf[:], self.ident_f[:])
            S.copy(skT[:, hp, :], bk[0][:, 0:128], eng="act")
        qhT = self.sb("qhT", [128, 16, 128], BF16)
        S_sb = self.sb("S_sb", [128, 16, 128], F32)
        S2 = self.sb("S2", [128, 16, 128], F32)
        v16 = self.sb("v16", [128, 16, 16], F32)
        i16 = self.sb("i16", [128, 16, 16], U32)
        itf = self.sb("itf", [128, 16, 16], F32)
        cand = self.sb("cand", [128, 8, 256], F32)
        cand2 = self.sb("cand2", [128, 8, 256], F32)
        cs = self.sb("cs", [128, 8, 16], F32)
        cp = self.sb("cp", [128, 8, 16], U32)
        cpi = self.sb("cpi", [128, 8, 16], U32)
        kf = [self.sb("kf%d" % i, [128, 128], F32) for i in range(2)]
        ee = self.sb("ee", [128, 8, 16], F32)
        zz = self.sb("zz", [128, 8], F32)
        gg = self.sb("gg", [128, 8, 16], F32)
        oh = self.sb("oh", [128, 128, 16], F32)
        sel = [self.sb("sel%d" % i, [128, 128], F32) for i in range(2)]
        nexp = self.nexp_chunks
        iota16 = self.iota_free.v(self.iota_free.t[:, 0:16].unsqueeze(1).to_broadcast([128, 128, 16]))

        def top16(vals_out, idx_out, src, scratch):
            S.op("dve", lambda e: e.max(vals_out.ap[:, 0:8], src.ap), reads=[src], writes=[vals_out])
            S.op("dve", lambda e: e.max_index(idx_out.ap[:, 0:8], vals_out.ap[:, 0:8], src.ap),
                 reads=[src, vals_out], writes=[idx_out])
            S.op("dve", lambda e: e.match_replace(scratch.ap, vals_out.ap[:, 0:8], src.ap, -1e30),
                 reads=[src, vals_out], writes=[scratch])
            S.op("dve", lambda e: e.max(vals_out.ap[:, 8:16], scratch.ap), reads=[scratch], writes=[vals_out])
            S.op("dve", lambda e: e.max_index(idx_out.ap[:, 8:16], vals_out.ap[:, 8:16], scratch.ap),
                 reads=[scratch, vals_out], writes=[idx_out])

        nexp = self.nexp_chunks
        ufc = [self.sb("ufc%d" % i, [128, D], F32) for i in range(4)]
        utc = [self.sb("utc%d" % i, [128, 8, 128], BF16) for i in range(2)]
        u3c = self.peer_u.t.rearrange("(i e) d -> i e d", e=128)
        conv_per_tile = (nexp + ns - 1) // ns

        def conv_issue(ci):
            if ci < nexp:
                S.dma(ufc[ci % 4][:], V(u3c[ci], "peer_u"), eng="sp")

        def conv_do(ci):
            for hf in range(2):
                pt = bk[6 + hf]
                for k in range(4):
                    c = hf * 4 + k
                    S.tr(pt[:, k * 128:(k + 1) * 128], ufc[ci % 4][:, c * 128:(c + 1) * 128], self.ident_f[:])
                S.copy(utc[ci % 2][:, hf * 4:(hf + 1) * 4, :],
                       pt.v(pt.t[:].rearrange("p (k e) -> p k e", e=128)), eng="act")
            S.dma(V(self.UT.t[ci], "UT"), utc[ci % 2][:], eng="act")

        for ci in range(3):
            conv_issue(ci)
        qhT2 = [qhT, self.sb("qhT_b", [128, 16, 128], BF16)]
        S_sb2 = [S_sb, self.sb("S_sb_b", [128, 16, 128], F32)]

        def front(m, i):
                tsl = slice(m * 128, (m + 1) * 128)
                for q4 in range(4):
                    for k in range(4):
                        hp = q4 * 4 + k
                        for c in range(8):
                            S.mm(bk[q4][:, k * 128:(k + 1) * 128], wpq[:, c, hp * 128:(hp + 1) * 128],
                                 self.xn2T[:, c, tsl], start=(c == 0), stop=(c == 7))
                    S.copy(qhT2[m % 2][:, q4 * 4:(q4 + 1) * 4, :],
                           bk[q4].v(bk[q4].t[:].rearrange("p (k t) -> p k t", t=128)), eng="act")
                for q4 in range(4):
                    for k in range(4):
                        hp = q4 * 4 + k
                        S.mm(bk[4 + q4][:, k * 128:(k + 1) * 128], qhT2[m % 2][:, hp, :], skT[:, hp, :])
                    S.copy(S_sb2[m % 2][:, q4 * 4:(q4 + 1) * 4, :],
                           bk[4 + q4].v(bk[4 + q4].t[:].rearrange("p (k t) -> p k t", t=128)), eng="act")

        def back(m, i):
                tsl = slice(m * 128, (m + 1) * 128)
                for hp in range(16):
                    top16(v16[:, hp, :], i16[:, hp, :], S_sb2[m % 2][:, hp, :], S2[:, hp, :])
                S.copy(itf[:], i16[:])
                v4 = v16.t[:].rearrange("p (h two) k -> p h two k", two=2)
                S.tt(cand.v(cand.t[:].rearrange("p h (a b) -> p h a b", b=16)),
                     v16.v(v4[:, :, 0, :].unsqueeze(3).to_broadcast([128, 8, 16, 16])),
                     v16.v(v4[:, :, 1, :].unsqueeze(2).to_broadcast([128, 8, 16, 16])), ALU.add)
                for h in range(8):
                    top16(cs[:, h, :], cp[:, h, :], cand[:, h, :], cand2[:, h, :])
                S.tt(ee[:], cs[:], cs.v(cs.t[:, :, 0:1].to_broadcast([128, 8, 16])), ALU.subtract)
                S.act(ee[:], ee[:], AF.Exp)
                S.red(zz[:], ee[:], ALU.add)
                S.recip(zz[:], zz[:])
                S.tt(gg[:], ee[:], zz.v(zz.t[:].unsqueeze(2).to_broadcast([128, 8, 16])), ALU.mult)
                it4 = itf.t[:].rearrange("p (h two) k -> p h two k", two=2)
                for w_, (op_, imm) in enumerate(((ALU.logical_shift_right, 4), (ALU.bitwise_and, 15))):
                    S.ts(cpi[:], cp[:], imm, op_)
                    S.copy(kf[w_][:], cpi.v(cpi.t[:].rearrange("p h k -> p (h k)")))
                    S.tt(oh[:], iota16, kf[w_].v(kf[w_].t[:].unsqueeze(2).to_broadcast([128, 128, 16])), ALU.is_equal)
                    S.tt(oh.v(oh.t[:].rearrange("p (h k) a -> p h k a", k=16)),
                         oh.v(oh.t[:].rearrange("p (h k) a -> p h k a", k=16)),
                         itf.v(it4[:, :, w_, :].unsqueeze(2).to_broadcast([128, 8, 16, 16])), ALU.mult)
                    S.red(sel[w_][:], oh[:], ALU.add)
                for w_, src in enumerate((sel[0][:], sel[1][:], gg.v(gg.t[:].rearrange("p h k -> p (h k)")))):
                    S.tr(bk[w_][:, 0:128], src, self.ident_f[:])
                    S.copy(selT[w_][:, tsl], bk[w_][:, 0:128], eng="act")
                for ci in range(m * conv_per_tile, min(nexp, (m + 1) * conv_per_tile)):
                    conv_issue(ci + 3)
                    conv_do(ci)

        skew(S, list(range(ns)), [front, back])
        self.release(mkA)
        TG = 256
        G_all = self.sb("G_all", [128, TG, 128], BF16)
        CH = 16
        P1c = [self.sb("P1c%d" % i, [128, CH, 128], BF16) for i in range(2)]
        P2c = [self.sb("P2c%d" % i, [128, CH, 128], BF16) for i in range(2)]
        eqc = [self.sb("eqc%d" % i, [128, CH, 128], BF16) for i in range(2)]
        ut = [self.sb("ut%d" % i, [128, 8, 128], BF16) for i in range(5)]
        vb = [self.sb("vb%d" % i, [128, D], BF16) for i in range(5)]
        a_sb = [self.sb("a_sb%d" % i, [128, TG], F32) for i in range(2)]
        h_bf = [self.sb("h_bf%d" % i, [128, TG], BF16) for i in range(2)]
        gF = self.sb("gF", [128, D], F32)
        S.dma(gF[:], V(self.final_norm_g.t.partition_broadcast(128), "final_norm_g"), eng="sp")
        h2 = [self.sb("h2_%d" % i, [128, D], F32) for i in range(1)]
        junk = self.sb("junkp", [128, D], BF16)
        ss = self.sb("ssp", [128, 1], F32)
        rstd = self.sb("rstdp", [128, 1], F32)
        iota3 = self.iota_free.v(self.iota_free.t[:].unsqueeze(1).to_broadcast([128, CH, 128]))
        u3 = self.peer_u.t.rearrange("(i e) d -> i e d", e=128)
        v3 = self.peer_v.t.rearrange("(i e) d -> i e d", e=128)
        for ps_ in range(ntok // TG):
            t0 = ps_ * TG
            ng = 0
            for c0 in range(0, TG, CH):
                tcs = slice(t0 + c0, t0 + c0 + CH)
                bc = lambda w_: selT[w_].v(selT[w_].t[:, tcs].unsqueeze(2).to_broadcast([128, CH, 128]))
                cpar = (c0 // CH) % 2
                S.tt(P2c[cpar][:], iota3, bc(1), ALU.is_equal)
                S.tt(eqc[cpar][:], iota3, bc(0), ALU.is_equal)
                S.tt(P1c[cpar][:], eqc[cpar][:], bc(2), ALU.mult)
                for k4 in range(CH // 4):
                    pb = bk[4 + ng % 2]
                    ng += 1
                    for k in range(4):
                        tk = k4 * 4 + k
                        S.mm(pb[:, k * 128:(k + 1) * 128], P2c[cpar][:, tk, :], P1c[cpar][:, tk, :])
                    S.copy(G_all[:, c0 + k4 * 4:c0 + k4 * 4 + 4, :],
                           pb.v(pb.t[:].rearrange("p (k i) -> p k i", i=128)), eng="act")
            def stL0(i, n):
                b = n % 5
                S.dma(ut[b][:], V(self.UT.t[i], "UT"), eng="sp")
                S.dma(vb[b][:], self.VB[i * 128:(i + 1) * 128, :], eng="act")

            def stA(i, n):
                b = n % 2
                pa = bk[4 + n % 2]
                for c in range(8):
                    S.mm(pa[:, 0:TG], ut[n % 5][:, c, :], self.xn2T[:, c, t0:t0 + TG], start=(c == 0), stop=(c == 7))
                S.act(a_sb[b][:], pa[:, 0:TG], AF.Gelu)
                S.tt(h_bf[b][:], a_sb[b][:], G_all[:, :, i], ALU.mult)

            def stV(i, n):
                b = n % 2
                for s_ in range(TG // 128):
                    for hf in range(2):
                        S.mm(bk[2 * s_ + hf][:], h_bf[b][:, s_ * 128:(s_ + 1) * 128], vb[n % 5][:, hf * 512:(hf + 1) * 512],
                             start=(n == 0), stop=(n == nexp - 1))

            skew(S, list(range(nexp)), [stL0, None, None, stA, stV])
            for s_ in range(TG // 128):
                m = ps_ * (TG // 128) + s_
                b = 0
                S.dma(h2[b][:], self.H1[m * 128:(m + 1) * 128, :], eng="sp")
                for hf in range(2):
                    S.tt(h2[b][:, hf * 512:(hf + 1) * 512], h2[b][:, hf * 512:(hf + 1) * 512], bk[2 * s_ + hf][:], ALU.add)
                S.act(junk[:], h2[b][:], AF.Square, accum=ss[:])
                S.act(rstd[:], ss[:], AF.Ln, bias=self.eps_t[:], scale=1.0 / D)
                S.act(rstd[:], rstd[:], AF.Exp, scale=-0.5)
                S.stt(h2[b][:], h2[b][:], rstd[:], gF[:], ALU.mult, ALU.mult)
                S.dma(self.out[m * 128:(m + 1) * 128, :], h2[b][:], eng="act")

    def sb_head(self, kt_, va_, qt_, hh, sbm, e_sb, sp_bf, t1, w_bf, R_sb, osb, pz, pC, pR, pO):
        S = self.S
        items = [(m, g) for m in range(self.ns) for g in range(m, -1, -1)]

        def stA(it, i):
            m, g = it
            k = i % 2
            qv = V(qt_.t[:, m * 128:(m + 1) * 128], "%s/m%d" % (qt_.name, m))
            for a in range(4):
                kt = 4 * g + a
                S.mm(pz[i % 3][:, a * 128:(a + 1) * 128], kt_[:, kt * 128:(kt + 1) * 128], qv)
            S.act(e_sb[k][:], pz[i % 3][:], AF.Exp, scale=0.125)
            S.act(sp_bf[k][:], e_sb[k][:], AF.Ln, bias=1.0, scale=1.0)
            if g == m:
                S.tt(sp_bf[k][:], sp_bf[k][:], sbm[:], ALU.mult)

        def stB(it, i):
            m, g = it
            k = i % 2
            S.mm(pC[k][:], self.tri_bf[:], sp_bf[k][:], start=True, stop=False)
            for s_ in range(1, 4):
                S.mm(pC[k][:, 0:(4 - s_) * 128], self.ones_bf[:], sp_bf[k][:, s_ * 128:512],
                     start=False, stop=(s_ == 3))
            for a in range(4):
                S.mm(pR[:, 0:128], self.ones_bf[:], sp_bf[k][:, a * 128:(a + 1) * 128],
                     start=(a == 0), stop=(a == 3))
            if g == m:
                S.copy(t1[k][:], pC[k][:])
                S.stt(t1[k][:], pz[i % 3][:], 0.125, t1[k][:], ALU.mult, ALU.subtract)
                S.copy(R_sb[:], pR[:, 0:128])
            else:
                S.tt(t1[k].v(t1[k].t[:].rearrange("p (a q) -> p a q", q=128)),
                     pC[k].v(pC[k].t[:].rearrange("p (a q) -> p a q", q=128)),
                     R_sb.v(R_sb.t[:].unsqueeze(1).to_broadcast([128, 4, 128])), ALU.add)
                S.stt(t1[k][:], pz[i % 3][:], 0.125, t1[k][:], ALU.mult, ALU.subtract)
                S.tt(R_sb[:], R_sb[:], pR[:, 0:128], ALU.add)

        def stB2(it, i):
            m, g = it
            k = i % 2
            S.act(w_bf[k][:], t1[k][:], AF.Exp)
            if g == m:
                S.tt(w_bf[k][:], w_bf[k][:], sbm[:], ALU.mult)

        def stC(it, i):
            m, g = it
            k = i % 2
            po = pO[m % 2]
            for a in range(4):
                kt = 4 * g + a
                S.mm(po[:, 0:DH], w_bf[k][:, a * 128:(a + 1) * 128], va_[:, kt, 0:DH],
                     start=(g == m and a == 0), stop=(g == 0 and a == 3))
            if g == 0:
                S.copy(osb[:, m, hh * DH:(hh + 1) * DH], po[:, 0:DH], eng="dve")

        skew(S, items, [stA, stB, stB2, stC])

    def moba_head(self, mh, kt_, va_, qt_, hh, osb, pz, pO, t1, w_bf, pTr):
        S = self.S
        h = mh
        slope = 2.0 ** (-(h + 1))
        cv = self.cv
        S.dma(self.kmf[:], V(self.KM.t[(h % 2) * DH:(h % 2 + 1) * DH, h // 2, :], "KM"), eng="sp")
        S.memset(self.kmb[:], 0.0)
        S.copy(self.kmb[0:DH, 0:32], self.kmf[:])
        pbs = self.banks[4]

        def qsub(m):
            return "%s/m%d" % (qt_.name, m)

        def gate1(m):
            nbc = 2 * m + 1
            S.mm(pbs[:, 0:nbc], V(qt_.t[:, m * 128:(m + 1) * 128], qsub(m)), self.kmb[:, 0:nbc])
            S.memset(self.bsm[:], -1e30)
            S.copy(self.bsm[:, 0:nbc], pbs[:, 0:nbc])
            S.ts(self.bsm[:, 2 * m:2 * m + 1], self.bsm[:, 2 * m:2 * m + 1], cv[:, 0:1], ALU.add)
            S.op("dve", lambda e, w=max(8, nbc): e.max(self.v8.t[:], self.bsm.t[:, 0:w]),
                 reads=[self.bsm[:]], writes=[self.v8[:]])
            S.ts(self.thr[:], self.v8[:, 2:3], -1e29, ALU.max)
            S.memset(self.selb[:], 0.0)
            S.ts(self.selb[:, 0:nbc], self.bsm[:, 0:nbc], self.thr[:], ALU.is_lt, NEG, ALU.mult)
            S.ts(self.selb[:, 2 * m:2 * m + 1], self.selb[:, 2 * m:2 * m + 1], cv[:, 1:2], ALU.mult)
            nb = 2 * m + 2
            S.ts(self.offm[:, 0:nb], self.iota_free[:, 0:nb], float(-2 * m), ALU.add, 2048.0 * slope, ALU.mult)
            S.memset(self.MBt[:], 0.0)
            S.tt(self.MBt[:, 68:68 + nb], self.offm[:, 0:nb], self.selb[:, 0:nb], ALU.add)
            S.memset(self.MBt[:, 64:65], 8.0 * slope)
            S.memset(self.MBt[:, 65:66], 1024.0 * slope)
            S.ts(self.MBt[:, 66:67], self.iota_part[:], -8.0 * slope, ALU.mult)
            S.copy(self.MBt[:, 67:68], cv[:, 8 + h:9 + h])

        def gate2(m):
            S.tr(pTr, self.MBt[:], self.ident_bf[:])
            S.copy(V(qt_.t[64:128, m * 128:(m + 1) * 128], qsub(m)),
                   V(self.banks_bf[4].t[64:128, 512:640], "bank4"), eng="act")

        gate1(0)
        gate2(0)
        items = [(m, g) for m in range(self.ns) for g in range(m, -1, -1)]

        def stA(it, i):
            m, g = it
            k = i % 2
            k3 = i % 3
            qv = V(qt_.t[:, m * 128:(m + 1) * 128], qsub(m))
            for a in range(4):
                kt = 4 * g + a
                S.mm(pz[k3][:, a * 128:(a + 1) * 128], kt_[:, kt * 128:(kt + 1) * 128], qv)
            if g == m:
                S.tt(t1[k][:], pz[k3][:], self.mbn[:], ALU.add)
                S.act(w_bf[k3][:], t1[k][:], AF.Exp, scale=0.125)
            else:
                S.act(w_bf[k3][:], pz[k3][:], AF.Exp, scale=0.125)
            if g == m and m + 1 < self.ns:
                gate1(m + 1)
            if g == 0 and m + 1 < self.ns:
                gate2(m + 1)

        def stB(it, i):
            m, g = it
            k3 = i % 3
            po = pO[m % 2]
            for a in range(4):
                kt = 4 * g + a
                S.mm(po[:, 0:DH + 1], w_bf[k3][:, a * 128:(a + 1) * 128], va_[:, kt, :],
                     start=(g == m and a == 0), stop=(g == 0 and a == 3))
            if g == 0:
                S.copy(self.den[:], po[:, DH:DH + 1])
                S.recip(self.den[:], self.den[:])
                S.ts(osb[:, m, hh * DH:(hh + 1) * DH], po[:, 0:DH], self.den[:], ALU.mult)

        skew(S, items, [stA, None, stB])


def core_consts(j):
    kl = np.arange(128)[:, None]
    ql = np.arange(128)[None, :]
    sbm = np.zeros((128, 4, 128), np.float32)
    mbn = np.full((128, 4, 128), NEG, np.float32)
    for i in range(4):
        if i < j:
            sbm[:, i, :] = 1.0
        elif i == j:
            sbm[:, i, :] = (kl < ql)
    own = (0, 1) if j < 2 else (2, 3)
    for i in range(4):
        if j >= 2 and i < 2:
            mbn[:, i, :] = 0.0
        elif i in own:
            if i < j:
                mbn[:, i, :] = 0.0
            elif i == j:
                mbn[:, i, :] = np.where(kl <= ql, 0.0, NEG)
    kc = np.zeros((128, NT_ALL, 128), np.float32)
    kc[64] = np.arange(128)[None, :]
    for kt in range(NT_ALL):
        kc[65, kt, :] = kt % 2
        kc[66, kt, :] = 1.0
        kc[67, kt, :] = 1.0
        kc[68 + kt // 2, kt, :] = 1.0
    cvec = np.zeros((128, 16), np.float32)
    cvec[:, 0] = -1e30 if j < 2 else 0.0
    cvec[:, 1] = 0.0 if j < 2 else 1.0
    for h in range(8):
        cvec[:, 8 + h] = -1024.0 * (2.0 ** (-(h + 1))) * j
    return {"sbm01": sbm.reshape(128, 512), "mbneg": mbn.reshape(128, 512), "cvec": cvec,
            "kcst": kc.reshape(128, S_LEN)}


def host_inputs(inputs):
    x = np.asarray(inputs["x"], np.float32)
    maps = []
    for c in range(8):
        b, j = c // 4, c % 4
        xall = np.ascontiguousarray(x[b])
        tiles = xall.reshape(NT_ALL, 128, D)
        xown = np.ascontiguousarray(tiles[[4 * m + j for m in range(NT_OWN)]].reshape(NT_OWN * 128, D))
        m = {
            "xall": xall,
            "xown": xown,
            "norm1_g": np.ascontiguousarray(np.asarray(inputs["norm1_g"], np.float32)[0]),
            "w_in": np.ascontiguousarray(np.asarray(inputs["w_in"], np.float32)[0]),
        }
        for k in ("w_out_moba", "w_out_sb", "w_mix_out", "norm2_g", "peer_w_q", "peer_u", "peer_v"):
            m[k] = np.ascontiguousarray(np.asarray(inputs[k], np.float32)[0])
        m["peer_sub_keys"] = np.ascontiguousarray(np.asarray(inputs["peer_sub_keys"], np.float32)[0].reshape(16, 128, 128))
        m["final_norm_g"] = np.ascontiguousarray(np.asarray(inputs["final_norm_g"], np.float32))
        m.update(core_consts(j))
        maps.append(m)
    return maps


_CACHE = {}


def kernel(**inputs):
    if "nc" not in _CACHE:
        _CACHE["nc"] = Builder().build()
    nc = _CACHE["nc"]
    maps = host_inputs(inputs)
    res = run_bass_kernel_spmd(nc, maps, core_ids=list(range(8)))
    out = np.zeros((2, S_LEN, D), np.float32)
    for c in range(8):
        b, j = c // 4, c % 4
        o = np.asarray(res.results[c]["out"]).reshape(NT_OWN, 128, D)
        ov = out[b].reshape(NT_ALL, 128, D)
        for m in range(NT_OWN):
            ov[4 * m + j] = o[m]
    return out
```

```python
import contextlib
import numpy as np
import concourse.bass as bass
import concourse.mybir as mybir
from concourse.bass_utils import run_bass_kernel_spmd

F32 = mybir.dt.float32
BF16 = mybir.dt.bfloat16
U32 = mybir.dt.uint32
I32 = mybir.dt.int32
AF = mybir.ActivationFunctionType
ALU = mybir.AluOpType
AX = mybir.AxisListType

S_LEN = 8192
D = 1024
NT_ALL = 64
NT_OWN = 16
DH = 64
NEG = -240000.0
EPS = 1e-6

ENGINES = ("pe", "act", "dve", "pool", "sp")
N_DMA_SEMS = 48


class V:
    __slots__ = ("ap", "res")

    def __init__(self, ap, res):
        self.ap = ap
        self.res = res if isinstance(res, tuple) else (res,)


class T:
    def __init__(self, t, name):
        self.t = t
        self.name = name

    def __getitem__(self, idx):
        return V(self.t[idx], self.name)

    def v(self, ap, sub=None):
        return V(ap, self.name if sub is None else self.name + "/" + sub)


class Op:
    __slots__ = ("eng", "fn", "deps", "idx", "is_dma", "signal", "count", "sem")


def _res(xs):
    out = []
    for x in xs:
        if x is None:
            continue
        if isinstance(x, V):
            out.extend(x.res)
        elif isinstance(x, str):
            out.append(x)
        elif isinstance(x, (int, float)):
            continue
        else:
            out.extend(x)
    return out


class Sched:
    def __init__(self, nc, same_engine_sync=True):
        self.nc = nc
        self.q = {e: [] for e in ENGINES}
        self.last_w = {}
        self.readers = {}
        self.same_engine_sync = same_engine_sync
        self.n_dma = 0
        self.dma_ops = []
        self.sw_dma_ops = []

    def op(self, eng, fn, reads=(), writes=(), dma=False, extra_deps=()):
        o = Op()
        o.eng = eng
        o.fn = fn
        o.is_dma = dma
        o.signal = False
        o.count = None
        o.sem = None
        o.idx = -1
        deps = []
        rres = _res(reads)
        wres = _res(writes)
        for r in rres:
            lw = self.last_w.get(r)
            if lw is not None:
                deps.append(lw)
        for w in wres:
            lw = self.last_w.get(w)
            if lw is not None:
                deps.append(lw)
            deps.extend(self.readers.get(w, ()))
        deps.extend(extra_deps)
        seen = set()
        fdeps = []
        for d in deps:
            if id(d) in seen or d is o:
                continue
            seen.add(id(d))
            if (not d.is_dma) and d.eng == eng:
                if eng == "pe" or not self.same_engine_sync:
                    continue
            fdeps.append(d)
        o.deps = fdeps
        for d in fdeps:
            d.signal = True
        for w in wres:
            self.last_w[w] = o
            self.readers[w] = []
        for r in rres:
            self.readers.setdefault(r, []).append(o)
        if dma and eng == "pool":
            o.signal = True
            o.idx = -2
            self.sw_dma_ops.append(o)
        elif dma:
            o.signal = True
            o.idx = self.n_dma
            self.n_dma += 1
            self.dma_ops.append(o)
        self.q[eng].append(o)
        return o

    def barrier(self):
        lasts = []
        for e in ENGINES:
            for o in reversed(self.q[e]):
                if not o.is_dma:
                    lasts.append(o)
                    break
        lasts.extend(self.dma_ops[-N_DMA_SEMS:])
        lasts.extend(self.sw_dma_ops)
        for e in ENGINES:
            self.op(e, lambda eng: eng.nop(), extra_deps=lasts)
        self.last_w = {}
        self.readers = {}

    def mm(self, out, lhsT, rhs, start=True, stop=True):
        return self.op("pe", lambda e: e.matmul(out.ap, lhsT.ap, rhs.ap, start=start, stop=stop),
                       reads=[lhsT, rhs] + ([] if start else [out]), writes=[out])

    def tr(self, out, in_, ident):
        return self.op("pe", lambda e: e.transpose(out.ap, in_.ap, ident.ap),
                       reads=[in_, ident], writes=[out])

    def act(self, out, in_, func, bias=0.0, scale=1.0, accum=None, eng="act"):
        b = bias.ap if isinstance(bias, V) else float(bias)
        s = scale.ap if isinstance(scale, V) else float(scale)
        kw = {}
        if accum is not None:
            kw["accum_out"] = accum.ap
        return self.op(eng, lambda e: e.activation(out.ap, in_.ap, func, bias=b, scale=s, **kw),
                       reads=[in_, bias, scale], writes=[out, accum])

    def tt(self, out, in0, in1, op, eng="dve"):
        return self.op(eng, lambda e: e.tensor_tensor(out.ap, in0.ap, in1.ap, op),
                       reads=[in0, in1], writes=[out])

    def ts(self, out, in0, s1, op0, s2=None, op1=None, eng="dve", accum=None):
        a1 = s1.ap if isinstance(s1, V) else s1
        a2 = s2.ap if isinstance(s2, V) else s2
        kw = {}
        if op1 is not None:
            kw["op1"] = op1
        if accum is not None:
            kw["accum_out"] = accum.ap
        return self.op(eng, lambda e: e.tensor_scalar(out.ap, in0.ap, a1, a2, op0, **kw),
                       reads=[in0, s1, s2], writes=[out, accum])

    def stt(self, out, in0, scalar, in1, op0, op1):
        a = scalar.ap if isinstance(scalar, V) else scalar
        return self.op("dve", lambda e: e.scalar_tensor_tensor(out.ap, in0.ap, a, in1.ap, op0, op1),
                       reads=[in0, scalar, in1], writes=[out])

    def copy(self, out, in_, eng="dve"):
        if eng == "act":
            return self.op("act", lambda e: e.copy(out.ap, in_.ap), reads=[in_], writes=[out])
        return self.op(eng, lambda e: e.tensor_copy(out.ap, in_.ap), reads=[in_], writes=[out])

    def red(self, out, in_, op, axis=AX.X, eng="dve"):
        return self.op(eng, lambda e: e.tensor_reduce(out.ap, in_.ap, axis, op),
                       reads=[in_], writes=[out])

    def memset(self, out, val, eng="dve"):
        return self.op(eng, lambda e: e.memset(out.ap, val), writes=[out])

    def recip(self, out, in_):
        return self.op("dve", lambda e: e.reciprocal(out.ap, in_.ap), reads=[in_], writes=[out])

    def dma(self, out, in_, eng="sp", **kw):
        return self.op(eng, lambda e: e.dma_start(out.ap, in_.ap, **kw),
                       reads=[in_], writes=[out], dma=True)

    def emit(self, stack):
        nc = self.nc
        sems = {e: stack.enter_context(nc.semaphore("s_" + e)) for e in ENGINES}
        dsems = [stack.enter_context(nc.semaphore("d%d" % i)) for i in range(N_DMA_SEMS)]
        swsems = [stack.enter_context(nc.semaphore("w%d" % i)) for i in range(len(self.sw_dma_ops))]
        for i, o in enumerate(self.sw_dma_ops):
            o.sem = swsems[i]
            o.count = 16
        for e in ENGINES:
            c = 0
            for o in self.q[e]:
                if o.is_dma and o.idx == -2:
                    continue
                if o.is_dma:
                    o.sem = dsems[o.idx % N_DMA_SEMS]
                    o.count = 16 * (o.idx // N_DMA_SEMS + 1)
                elif o.signal:
                    c += 1
                    o.sem = sems[e]
                    o.count = c
        final_waits = list(self.dma_ops[-N_DMA_SEMS:]) + list(self.sw_dma_ops)
        block = stack.enter_context(nc.Block())
        engmap = {"pe": block.tensor, "act": block.scalar, "dve": block.vector,
                  "pool": block.gpsimd, "sp": block.sync}
        for e in ENGINES:
            ops = self.q[e]

            def body(eng, ops=ops, e=e):
                waited = {}

                def wait(d):
                    key = id(d.sem)
                    if waited.get(key, 0) >= d.count:
                        return
                    eng.wait_ge(d.sem, d.count)
                    waited[key] = d.count

                for o in ops:
                    if o.is_dma and o.idx >= N_DMA_SEMS:
                        wait(self.dma_ops[o.idx - N_DMA_SEMS])
                    for d in o.deps:
                        wait(d)
                    ins = o.fn(eng)
                    if o.signal:
                        ins.then_inc(o.sem, 16 if o.is_dma else 1)
                if e == "sp":
                    for d in final_waits:
                        wait(d)

            engmap[e](body)


SAME_ENGINE_SYNC = True
SBUF_BASE = 16384
SBUF_TOP = 192 * 1024


def skew(S, items, stages):
    n, ns = len(items), len(stages)
    for step in range(n + ns - 1):
        for st in range(ns):
            i = step - st
            if 0 <= i < n and stages[st] is not None:
                stages[st](items[i], i)


class Builder:
    def __init__(self, ns=NT_OWN, dbg=(), upto=99, nexp=128):
        self.ns = ns
        self.nkt = min(NT_ALL, 4 * ns)
        self.upto = upto
        self.dbg = set(dbg)
        self.nc = bass.Bass("TRN2", target_bir_lowering=False)
        self.S = Sched(self.nc, same_engine_sync=SAME_ENGINE_SYNC)
        self.top = SBUF_BASE
        self.nexp_chunks = nexp
        self.uid = 0

    def sb(self, name, shape, dt=F32):
        esz = 2 if dt == BF16 else 4
        n = 1
        for x in shape[1:]:
            n *= x
        nbytes = (n * esz + 63) // 64 * 64
        off = self.top
        self.top += nbytes
        self.hw = max(getattr(self, 'hw', 0), self.top)
        assert self.top <= SBUF_TOP, ("SBUF overflow", name, self.top)
        self.uid += 1
        nm = "%s_%d" % (name, self.uid)
        t = self.nc.alloc_sbuf_tensor_at(nm, list(shape), dt, offset=off)
        return T(t, nm)

    def mark(self):
        return self.top

    def release(self, m):
        self.S.barrier()
        self.top = m

    def dram(self, name, shape, dt, kind="Internal"):
        if name in self.dbg:
            kind = "ExternalOutput"
        t = self.nc.dram_tensor(name, list(shape), dt, kind=kind)
        return T(t.ap(), name)

    def bank(self, i, shape=None, dt=F32):
        return self.banks[i]

    def build(self):
        S = self.S
        stack = contextlib.ExitStack()
        with stack:
            self.banks = []
            self.banks_bf = []
            for i in range(8):
                t = self.nc.alloc_psum_tensor("bank%d" % i, [128, 512], F32)
                self.banks.append(T(t, "bank%d" % i))
                self.banks_bf.append(T(t[:].bitcast(BF16), "bank%d" % i))
            self.declare_io()
            self.consts()
            if self.upto >= 1:
                self.phase_q()
            if self.upto >= 2:
                self.phase_kv()
            if self.upto >= 3:
                self.phase_attn()
            if self.upto >= 5:
                self.xn2T = self.sb("xn2T", [128, 8, self.ns * 128], BF16)
                self.g2 = self.sb("g2", [128, 8], F32)
                S.dma(self.g2[:], V(self.norm2_g.t.rearrange("(c p) -> p c", p=128), "norm2_g"),
                      allow_slow_non_contiguous=True)
                self.phase_mix()
            if self.upto >= 6:
                self.phase_peer()
            S.emit(stack)
        return self.nc

    def declare_io(self):
        d = self.dram
        EI = "ExternalInput"
        self.xall = d("xall", [S_LEN, D], F32, EI)
        self.xown = d("xown", [NT_OWN * 128, D], F32, EI)
        self.norm1_g = d("norm1_g", [D], F32, EI)
        self.w_in = d("w_in", [D, 5120], F32, EI)
        self.sbm01 = d("sbm01", [128, 512], F32, EI)
        self.mbneg = d("mbneg", [128, 512], F32, EI)
        self.kcst = d("kcst", [128, S_LEN], F32, EI)
        self.cvec = d("cvec", [128, 16], F32, EI)
        self.w_out_moba = d("w_out_moba", [512, D], F32, EI)
        self.w_out_sb = d("w_out_sb", [512, D], F32, EI)
        self.w_mix_out = d("w_mix_out", [D, D], F32, EI)
        self.norm2_g = d("norm2_g", [D], F32, EI)
        self.peer_w_q = d("peer_w_q", [D, 2048], F32, EI)
        self.peer_sub_keys = d("peer_sub_keys", [16, 128, 128], F32, EI)
        self.peer_u = d("peer_u", [16384, D], F32, EI)
        self.peer_v = d("peer_v", [16384, D], F32, EI)
        self.final_norm_g = d("final_norm_g", [D], F32, EI)
        self.out = d("out", [NT_OWN * 128, D], F32, "ExternalOutput")
        self.H1 = d("H1", [NT_OWN * 128, D], F32)
        self.UT = d("UT", [128, 128, 8, 128], BF16)
        self.VB = d("VB", [16384, D], BF16)
        self.KT = d("KT", [16, DH, S_LEN], BF16)
        self.VS = d("VS", [S_LEN, 16 * DH], BF16)
        self.KM = d("KM", [128, 4, 32], F32)
        self.QT = d("QT", [16, DH, NT_OWN * 128], BF16)
        self.SG = d("SG", [NT_OWN * 128, 2048], F32)
        self.AT = d("AT", [128, 8, NT_OWN * 128], BF16)

    def consts(self):
        S = self.S
        self.ident_bf = self.sb("ident_bf", [128, 128], BF16)
        self.ident_f = self.sb("ident_f", [128, 128], F32)
        self.iota_free = self.sb("iota_free", [128, 128], F32)
        self.iota_part = self.sb("iota_part", [128, 1], F32)
        self.eps_t = self.sb("eps_t", [128, 1], F32)
        self.one_t = self.sb("one_t", [128, 1], F32)
        S.op("pool", lambda e: e.iota(self.iota_free.t[:], [[1, 128]], base=0, channel_multiplier=0,
                                      allow_small_or_imprecise_dtypes=True), writes=[self.iota_free[:]])
        S.op("pool", lambda e: e.iota(self.iota_part.t[:], [[0, 1]], base=0, channel_multiplier=1,
                                      allow_small_or_imprecise_dtypes=True), writes=[self.iota_part[:]])
        S.ts(self.ident_f[:], self.iota_free[:], self.iota_part[:], ALU.is_equal)
        S.copy(self.ident_bf[:], self.ident_f[:])
        S.memset(self.eps_t[:], EPS)
        S.memset(self.one_t[:], 1.0)
        self.tri_bf = self.sb("tri_bf", [128, 128], BF16)
        self.ones_bf = self.sb("ones_bf", [128, 128], BF16)
        S.ts(self.tri_bf[:], self.iota_free[:], self.iota_part[:], ALU.is_le)
        S.memset(self.ones_bf[:], 1.0)

    def rms_tile(self, xt, xn_bf, ss, rstd, junk, gB=None):
        S = self.S
        S.act(junk[:], xt[:], AF.Square, accum=ss[:])
        S.act(rstd[:], ss[:], AF.Ln, bias=self.eps_t[:], scale=1.0 / D)
        S.act(rstd[:], rstd[:], AF.Exp, scale=-0.5)
        if gB is None:
            S.ts(xn_bf[:], xt[:], rstd[:], ALU.mult)
        else:
            S.stt(xn_bf[:], xt[:], rstd[:], gB[:], ALU.mult, ALU.mult)

    def load_weights_cast(self, dst, src3, col_ranges):
        S = self.S
        o = 0
        for (c0, nc_) in col_ranges:
            S.dma(dst[:, :, o:o + nc_], V(src3[:, :, c0:c0 + nc_], "w_dram"), eng="pool")
            o += nc_

    def phase_q(self):
        S = self.S
        g1B = self.g1B = self.sb("g1B", [128, D], F32)
        S.dma(g1B[:], V(self.norm1_g.t.partition_broadcast(128), "norm1_g"), eng="sp")
        w3 = self.w_in.t.rearrange("(c p) n -> p c n", p=128)
        self.mk_wkv = self.mark()
        self.wkv = self.sb("wkv", [128, 8, 2048], BF16)
        mk2 = self.mark()
        wq = self.sb("wq", [128, 8, 1024], BF16)
        wg = self.sb("wg", [128, 8, 2048], BF16)
        self.load_weights_cast(wq, w3, [(0, 512), (1536, 512)])
        self.load_weights_cast(wg, w3, [(3072, 2048)])
        self.load_weights_cast(self.wkv, w3, [(512, 512), (2048, 512), (1024, 512), (2560, 512)])
        xt = [self.sb("xt%d" % i, [128, D], F32) for i in range(3)]
        xn = [self.sb("xn%d" % i, [128, D], BF16) for i in range(2)]
        junk = self.sb("junk", [128, D], BF16)
        ss = [self.sb("ss%d" % i, [128, 1], F32) for i in range(2)]
        rstd = [self.sb("rstd%d" % i, [128, 1], F32) for i in range(2)]
        xnT = [self.sb("xnT%d" % i, [128, 8, 128], BF16) for i in range(2)]
        qsb = [self.sb("qsb%d" % i, [128, 8, 128], BF16) for i in range(2)]
        sg = [self.sb("sg%d" % i, [128, 2048], F32) for i in range(2)]
        xo = self.xown.t.rearrange("(t p) d -> t p d", p=128)
        qt3 = self.QT.t.rearrange("(cc two) d t -> (two d) cc t", two=2)

        def s0(m, i):
            S.dma(xt[m % 3][:], V(xo[m], "xown"), eng="sp")

        def s1(m, i):
            b = m % 2
            self.rms_tile(xt[m % 3], xn[b], ss[b], rstd[b], junk, gB=self.g1B)

        def s2(m, i):
            b = m % 2
            pTb = self.banks_bf[b]
            for c in range(8):
                S.tr(pTb[:, c * 128:(c + 1) * 128], xn[b][:, c * 128:(c + 1) * 128], self.ident_bf[:])
            S.copy(xnT[b][:], pTb.v(pTb.t[:, 0:1024].rearrange("p (c t) -> p c t", t=128)), eng="dve")

        def s3(m, i):
            b = m % 2
            for half in range(2):
                pq = self.banks[2 + half]
                for k in range(4):
                    cc = half * 4 + k
                    for c in range(8):
                        S.mm(pq[:, k * 128:(k + 1) * 128], wq[:, c, cc * 128:(cc + 1) * 128], xnT[b][:, c, :],
                             start=(c == 0), stop=(c == 7))
                S.copy(qsb[b][:, half * 4:(half + 1) * 4, :],
                       pq.v(pq.t[:].rearrange("p (k t) -> p k t", t=128)), eng="act")
            S.dma(V(qt3[:, :, m * 128:(m + 1) * 128], "QT"), qsb[b][:], eng="act")
            for k in range(4):
                pg = self.banks[4 + k]
                for c in range(8):
                    S.mm(pg[:], xnT[b][:, c, :], wg[:, c, k * 512:(k + 1) * 512], start=(c == 0), stop=(c == 7))
                S.act(sg[b][:, k * 512:(k + 1) * 512], pg[:], AF.Exp, scale=-1.0)
            S.ts(sg[b][:], sg[b][:], 1.0, ALU.add)
            S.recip(sg[b][:], sg[b][:])
            S.dma(self.SG[m * 128:(m + 1) * 128, :], sg[b][:], eng="act")

        skew(S, list(range(self.ns)), [s0, None, s1, s2, s3])
        self.release(mk2)

    def phase_kv(self):
        S = self.S
        mk = self.mark()
        wkv = self.wkv
        xt = [self.sb("xt%d" % i, [128, D], F32) for i in range(3)]
        xn = [self.sb("xn%d" % i, [128, D], BF16) for i in range(2)]
        junk = self.sb("junk", [128, D], BF16)
        ss = [self.sb("ss%d" % i, [128, 1], F32) for i in range(2)]
        rstd = [self.sb("rstd%d" % i, [128, 1], F32) for i in range(2)]
        xnT = [self.sb("xnT%d" % i, [128, 8, 512], BF16) for i in range(2)]
        ksb = [self.sb("ksb%d" % i, [128, 512], BF16) for i in range(4)]
        vsb = [self.sb("vsb%d" % i, [128, 1024], BF16) for i in range(3)]
        kmT = self.sb("kmT", [128, 4, 32], F32)
        S.memset(kmT[:], 0.0)
        xa = self.xall.t.rearrange("(t p) d -> t p d", p=128)
        kt4 = self.KT.t.rearrange("(cc two) d t -> cc (two d) t", two=2)
        cnt = {"pk": 0, "ks": 0, "conv": 0}
        pK = [self.banks[2 + i] for i in range(4)]
        nexp = self.nexp_chunks
        bk = self.banks
        def xsub(cb, j):
            return "xnT%d/t%d" % (cb, j)

        def s0(tt, i):
            S.dma(xt[tt % 3][:], V(xa[tt], "xall"), eng="sp")

        def s1(tt, i):
            b = tt % 2
            self.rms_tile(xt[tt % 3], xn[b], ss[b], rstd[b], junk, gB=self.g1B)

        def s2(tt, i):
            b = tt % 2
            cb = (tt // 4) % 2
            j = tt % 4
            pTb = self.banks_bf[b]
            for c in range(8):
                S.tr(pTb[:, c * 128:(c + 1) * 128], xn[b][:, c * 128:(c + 1) * 128], self.ident_bf[:])
            S.copy(V(xnT[cb].t[:, :, j * 128:(j + 1) * 128], xsub(cb, j)),
                   pTb.v(pTb.t[:, 0:1024].rearrange("p (c t) -> p c t", t=128)), eng="dve")

        def s3(tt, i):
            b = tt % 2
            ch = tt // 4
            cb = ch % 2
            j = tt % 4
            for vh in range(2):
                pk = pK[cnt["pk"] % 4]
                cnt["pk"] += 1
                for c in range(8):
                    S.mm(pk[:], V(xnT[cb].t[:, c, j * 128:(j + 1) * 128], xsub(cb, j)),
                         wkv[:, c, 1024 + vh * 512:1024 + (vh + 1) * 512], start=(c == 0), stop=(c == 7))
                S.copy(vsb[tt % 3][:, vh * 512:(vh + 1) * 512], pk[:], eng="act")
            S.dma(self.VS[tt * 128:(tt + 1) * 128, :], vsb[tt % 3][:], eng="act")
            if j == 3:
                allsub = tuple(xsub(cb, jj) for jj in range(4))
                for cc in range(8):
                    pk = pK[cnt["pk"] % 4]
                    cnt["pk"] += 1
                    for c in range(8):
                        S.mm(pk[:], wkv[:, c, cc * 128:(cc + 1) * 128], V(xnT[cb].t[:, c, :], allsub),
                             start=(c == 0), stop=(c == 7))
                    kb = ksb[cnt["ks"] % 4]
                    cnt["ks"] += 1
                    if cc < 4:
                        for hb in range(2):
                            S.act(kb[:, hb * 256:(hb + 1) * 256], pk[:, hb * 256:(hb + 1) * 256], AF.Copy,
                                  accum=kmT[:, cc, ch * 2 + hb:ch * 2 + hb + 1])
                    else:
                        S.copy(kb[:], pk[:], eng="act")
                    S.dma(V(kt4[cc][:, ch * 512:(ch + 1) * 512], "KT"), kb[:], eng="act")

        skew(S, list(range(self.nkt)), [s0, None, s1, s2, s3])
        S.ts(kmT[:], kmT[:], 1.0 / 256.0, ALU.mult)
        S.dma(self.KM[:], kmT[:], eng="sp")
        self.release(self.mk_wkv)

    def phase_attn(self):
        S = self.S
        mk = self.mark()
        ns, nkt = self.ns, self.nkt
        ncol = ns * 128
        sbm = self.sb("sbm", [128, 512], F32)
        S.dma(sbm[:], self.sbm01[:], eng="sp")
        ktb = [self.sb("ktb%d" % i, [128, nkt * 128], BF16) for i in range(2)]
        vab = [self.sb("vab%d" % i, [128, nkt, DH + 1], BF16) for i in range(2)]
        qtb = [self.sb("qtb%d" % i, [128, ncol], BF16) for i in range(2)]
        for i in range(2):
            S.memset(vab[i][:], 1.0)
            S.memset(V(qtb[i].t[64:128, :], tuple("%s/m%d" % (qtb[i].name, mm_) for mm_ in range(ns))), 0.0)
        cv = self.cv = self.sb("cv", [128, 16], F32)
        S.dma(cv[:], self.cvec[:], eng="sp")
        mbn = self.mbn = self.sb("mbn", [128, 512], F32)
        S.dma(mbn[:], self.mbneg[:], eng="sp")
        self.kl_f = self.sb("kl_f", [128, nkt * 128], F32)
        S.dma(self.kl_f[:], self.kcst[:, 0:nkt * 128], eng="sp")
        for i in range(2):
            S.copy(ktb[i][64:128, :], self.kl_f[64:128, :], eng="dve")
        for r0 in range(0, self.nexp_chunks * 128, 2048):
            r1 = min(self.nexp_chunks * 128, r0 + 2048)
            S.dma(self.VB[r0:r1, :], self.peer_v[r0:r1, :], eng="pool")
        self.kmf = self.sb("kmf", [DH, 32], F32)
        self.kmb = self.sb("kmb", [128, 40], BF16)
        self.bsm = self.sb("bsm", [128, 40], F32)
        self.v8 = self.sb("v8", [128, 8], F32)
        self.thr = self.sb("thr", [128, 1], F32)
        self.selb = self.sb("selb", [128, 34], F32)
        self.offm = self.sb("offm", [128, 34], F32)
        self.MBt = self.sb("MBt", [128, 128], BF16)
        self.MBt2 = [self.MBt, self.sb("MBtb", [128, 128], BF16)]
        self.den = self.sb("den", [128, 1], F32)
        e_sb = [self.sb("e_sb%d" % i, [128, 512], F32) for i in range(2)]
        sp_bf = [self.sb("sp_bf%d" % i, [128, 512], BF16) for i in range(2)]
        t1 = [self.sb("t1_%d" % i, [128, 512], F32) for i in range(2)]
        w_bf = [self.sb("w_bf%d" % i, [128, 512], BF16) for i in range(3)]
        R_sb = self.sb("R_sb", [128, 128], F32)
        osb = self.sb("osb", [128, ns, 128], BF16)
        otb = [self.sb("otb%d" % i, [128, 128], BF16) for i in range(2)]
        pz = [self.banks[0], self.banks[1], self.banks[2]]
        pC = [self.banks[3], self.banks[7]]
        pR = self.banks[4]
        pO = [self.banks[5], self.banks[6]]
        pTr = V(self.banks_bf[4].t[:, 512:640], "bank4")
        nload = 0
        do_sb = self.upto >= 4 or self.upto == 3
        for mixer in ([1] if self.upto == 3 else ([0] if self.upto == 4 else [0, 1])):
            for hp in range(4):
                for hh in range(2):
                    mh = mixer * 8 + hp * 2 + hh
                    lb = nload % 2
                    nload += 1
                    kt_, va_, qt_ = ktb[lb], vab[lb], qtb[lb]
                    S.dma(kt_[0:DH, :], V(self.KT.t[mh][:, 0:nkt * 128], "KT"), eng="sp")
                    S.dma(va_[:, :, 0:DH], V(self.VS.t[0:nkt * 128, mh * DH:(mh + 1) * DH].rearrange("(t p) d -> p t d", p=128), "VS"), eng="sp")
                    qall = tuple("%s/m%d" % (qt_.name, mm_) for mm_ in range(ns))
                    S.dma(V(qt_.t[0:DH, :], qall), V(self.QT.t[mh][:, 0:ncol], "QT"), eng="sp")
                    if mixer == 1:
                        S.memset(V(qt_.t[64:128, :], qall), 0.0, eng="dve")
                    if mixer == 1:
                        self.sb_head(kt_, va_, qt_, hh, sbm, e_sb, sp_bf, t1, w_bf, R_sb, osb, pz, pC, pR, pO)
                    else:
                        self.moba_head(mh, kt_, va_, qt_, hh, osb, pz, pO, t1, w_bf, pTr)
                for m in range(ns):
                    ob = otb[m % 2]
                    S.tr(pTr, osb[:, m, :], self.ident_bf[:])
                    S.copy(ob[:], pTr, eng="act")
                    S.dma(V(self.AT.t[:, mixer * 4 + hp, m * 128:(m + 1) * 128], "AT"), ob[:], eng="act")
        self.release(mk)

    def phase_mix(self):
        S = self.S
        mk = self.mark()
        woa = self.sb("woa", [128, 4, 1024], BF16)
        wob = self.sb("wob", [128, 4, 1024], BF16)
        wmx = self.sb("wmx", [128, 8, 1024], BF16)
        for (dst, src) in ((woa, self.w_out_moba), (wob, self.w_out_sb), (wmx, self.w_mix_out)):
            self.load_weights_cast(dst, src.t.rearrange("(c p) n -> p c n", p=128), [(0, 1024)])
        at_sb = [self.sb("at_sb%d" % i, [128, 8, 128], BF16) for i in range(2)]
        sg = [self.sb("sgm%d" % i, [128, 2048], F32) for i in range(2)]
        xt = [self.sb("xtm%d" % i, [128, D], F32) for i in range(3)]
        tmp = self.sb("tmpm", [128, D], F32)
        mixed = [self.sb("mixed%d" % i, [128, D], BF16) for i in range(2)]
        mixT = self.sb("mixT", [128, 8, 128], BF16)
        h1 = [self.sb("h1_%d" % i, [128, D], F32) for i in range(2)]
        xn2 = self.sb("xn2", [128, D], BF16)
        junk = self.sb("junkm", [128, D], BF16)
        ss = self.sb("ssm", [128, 1], F32)
        rstd = self.sb("rstdm", [128, 1], F32)
        bk = self.banks

        def m0(m, i):
            b = m % 2
            S.dma(at_sb[b][:], V(self.AT.t[:, :, m * 128:(m + 1) * 128], "AT"), eng="sp")
            S.dma(sg[b][:], self.SG[m * 128:(m + 1) * 128, :], eng="sp")
            S.dma(xt[m % 3][:], self.xown[m * 128:(m + 1) * 128, :], eng="sp")

        def m1(m, i):
            b = m % 2
            for mixer, w in ((0, woa), (1, wob)):
                for hf in range(2):
                    for hp in range(4):
                        S.mm(bk[mixer * 2 + hf][:], at_sb[b][:, mixer * 4 + hp, :], w[:, hp, hf * 512:(hf + 1) * 512],
                             start=(hp == 0), stop=(hp == 3))
            for hf in range(2):
                cs = slice(hf * 512, (hf + 1) * 512)
                S.tt(tmp[:, cs], bk[hf][:], sg[b][:, cs], ALU.mult)
                S.tt(sg[b][:, 1024 + hf * 512:1024 + (hf + 1) * 512], bk[2 + hf][:],
                     sg[b][:, 1024 + hf * 512:1024 + (hf + 1) * 512], ALU.mult)
            S.tt(mixed[b][:], tmp[:], sg[b][:, 1024:2048], ALU.add)

        def m2(m, i):
            b = m % 2
            pT = self.banks_bf[4]
            for c in range(8):
                S.tr(pT[:, c * 128:(c + 1) * 128], mixed[b][:, c * 128:(c + 1) * 128], self.ident_bf[:])
            S.copy(mixT[:], pT.v(pT.t[:, 0:1024].rearrange("p (c t) -> p c t", t=128)), eng="act")
            for hf in range(2):
                for c in range(8):
                    S.mm(bk[5 + hf][:], mixT[:, c, :], wmx[:, c, hf * 512:(hf + 1) * 512], start=(c == 0), stop=(c == 7))
                S.tt(h1[b][:, hf * 512:(hf + 1) * 512], bk[5 + hf][:], xt[m % 3][:, hf * 512:(hf + 1) * 512], ALU.add)

        def m3(m, i):
            b = m % 2
            self.rms_tile(h1[b], xn2, ss, rstd, junk)
            S.dma(self.H1[m * 128:(m + 1) * 128, :], h1[b][:], eng="act")
            pT2 = self.banks_bf[7]
            for c in range(8):
                S.tr(pT2[:, c * 128:(c + 1) * 128], xn2[:, c * 128:(c + 1) * 128], self.ident_bf[:])
            for c in range(8):
                S.act(self.xn2T[:, c, m * 128:(m + 1) * 128], pT2[:, c * 128:(c + 1) * 128], AF.Copy,
                      scale=self.g2[:, c:c + 1])

        skew(S, list(range(self.ns)), [m0, m1, m2, m3])
        self.release(mk)

    def phase_peer(self):
        S = self.S
        ns = self.ns
        bk = self.banks
        ntok = ns * 128
        selT = [self.sb("selT%d" % i, [128, ntok], BF16 if i < 2 else F32) for i in range(3)]
        mkA = self.mark()
        wpq = self.sb("wpq", [128, 8, 2048], BF16)
        w3 = self.peer_w_q.t.rearrange("(c p) n -> p c n", p=128)
        self.load_weights_cast(wpq, w3, [(0, 2048)])
        skT = self.sb("skT", [128, 16, 128], BF16)
        skf = self.sb("skf", [128, 128], F32)
        for hp in range(16):
            S.dma(skf[:], self.peer_sub_keys[hp], eng="sp")
            S.tr(bk[0][:, 0:128], skf[:], self.ident_f[:])
            S.copy(skT[:, hp, :], bk[0][:, 0:128], eng="act")
        qhT = self.sb("qhT", [128, 16, 128], BF16)
        S_sb = self.sb("S_sb", [128, 16, 128], F32)
        S2 = self.sb("S2", [128, 16, 128], F32)
        v16 = self.sb("v16", [128, 16, 16], F32)
        i16 = self.sb("i16", [128, 16, 16], U32)
        itf = self.sb("itf", [128, 16, 16], F32)
        cand = self.sb("cand", [128, 8, 256], F32)
        cand2 = self.sb("cand2", [128, 8, 256], F32)
        cs = self.sb("cs", [128, 8, 16], F32)
        cp = self.sb("cp", [128, 8, 16], U32)
        cpi = self.sb("cpi", [128, 8, 16], U32)
        kf = [self.sb("kf%d" % i, [128, 128], F32) for i in range(2)]
        ee = self.sb("ee", [128, 8, 16], F32)
        zz = self.sb("zz", [128, 8], F32)
        gg = self.sb("gg", [128, 8, 16], F32)
        oh = self.sb("oh", [128, 128, 16], F32)
        sel = [self.sb("sel%d" % i, [128, 128], F32) for i in range(2)]
        nexp = self.nexp_chunks
        iota16 = self.iota_free.v(self.iota_free.t[:, 0:16].unsqueeze(1).to_broadcast([128, 128, 16]))

        def top16(vals_out, idx_out, src, scratch):
            S.op("dve", lambda e: e.max(vals_out.ap[:, 0:8], src.ap), reads=[src], writes=[vals_out])
            S.op("dve", lambda e: e.max_index(idx_out.ap[:, 0:8], vals_out.ap[:, 0:8], src.ap),
                 reads=[src, vals_out], writes=[idx_out])
            S.op("dve", lambda e: e.match_replace(scratch.ap, vals_out.ap[:, 0:8], src.ap, -1e30),
                 reads=[src, vals_out], writes=[scratch])
            S.op("dve", lambda e: e.max(vals_out.ap[:, 8:16], scratch.ap), reads=[scratch], writes=[vals_out])
            S.op("dve", lambda e: e.max_index(idx_out.ap[:, 8:16], vals_out.ap[:, 8:16], scratch.ap),
                 reads=[scratch, vals_out], writes=[idx_out])

        nexp = self.nexp_chunks
        ufc = [self.sb("ufc%d" % i, [128, D], F32) for i in range(4)]
        utc = [self.sb("utc%d" % i, [128, 8, 128], BF16) for i in range(2)]
        u3c = self.peer_u.t.rearrange("(i e) d -> i e d", e=128)
        conv_per_tile = (nexp + ns - 1) // ns

        def conv_issue(ci):
            if ci < nexp:
                S.dma(ufc[ci % 4][:], V(u3c[ci], "peer_u"), eng="sp")

        def conv_do(ci):
            for hf in range(2):
                pt = bk[6 + hf]
                for k in range(4):
                    c = hf * 4 + k
                    S.tr(pt[:, k * 128:(k + 1) * 128], ufc[ci % 4][:, c * 128:(c + 1) * 128], self.ident_f[:])
                S.copy(utc[ci % 2][:, hf * 4:(hf + 1) * 4, :],
                       pt.v(pt.t[:].rearrange("p (k e) -> p k e", e=128)), eng="act")
            S.dma(V(self.UT.t[ci], "UT"), utc[ci % 2][:], eng="act")

        for ci in range(3):
            conv_issue(ci)
        qhT2 = [qhT, self.sb("qhT_b", [128, 16, 128], BF16)]
        S_sb2 = [S_sb, self.sb("S_sb_b", [128, 16, 128], F32)]

        def front(m, i):
                tsl = slice(m * 128, (m + 1) * 128)
                for q4 in range(4):
                    for k in range(4):
                        hp = q4 * 4 + k
                        for c in range(8):
                            S.mm(bk[q4][:, k * 128:(k + 1) * 128], wpq[:, c, hp * 128:(hp + 1) * 128],
                                 self.xn2T[:, c, tsl], start=(c == 0), stop=(c == 7))
                    S.copy(qhT2[m % 2][:, q4 * 4:(q4 + 1) * 4, :],
                           bk[q4].v(bk[q4].t[:].rearrange("p (k t) -> p k t", t=128)), eng="act")
                for q4 in range(4):
                    for k in range(4):
                        hp = q4 * 4 + k
                        S.mm(bk[4 + q4][:, k * 128:(k + 1) * 128], qhT2[m % 2][:, hp, :], skT[:, hp, :])
                    S.copy(S_sb2[m % 2][:, q4 * 4:(q4 + 1) * 4, :],
                           bk[4 + q4].v(bk[4 + q4].t[:].rearrange("p (k t) -> p k t", t=128)), eng="act")

        def back(m, i):
                tsl = slice(m * 128, (m + 1) * 128)
                for ci in range(m * conv_per_tile, min(nexp, (m + 1) * conv_per_tile)):
                    conv_issue(ci + 3)
                    conv_do(ci)
                for hp in range(16):
                    top16(v16[:, hp, :], i16[:, hp, :], S_sb2[m % 2][:, hp, :], S2[:, hp, :])
                S.copy(itf[:], i16[:])
                v4 = v16.t[:].rearrange("p (h two) k -> p h two k", two=2)
                S.tt(cand.v(cand.t[:].rearrange("p h (a b) -> p h a b", b=16)),
                     v16.v(v4[:, :, 0, :].unsqueeze(3).to_broadcast([128, 8, 16, 16])),
                     v16.v(v4[:, :, 1, :].unsqueeze(2).to_broadcast([128, 8, 16, 16])), ALU.add)
                for h in range(8):
                    top16(cs[:, h, :], cp[:, h, :], cand[:, h, :], cand2[:, h, :])
                S.tt(ee[:], cs[:], cs.v(cs.t[:, :, 0:1].to_broadcast([128, 8, 16])), ALU.subtract)
                S.act(ee[:], ee[:], AF.Exp)
                S.red(zz[:], ee[:], ALU.add)
                S.recip(zz[:], zz[:])
                S.tt(gg[:], ee[:], zz.v(zz.t[:].unsqueeze(2).to_broadcast([128, 8, 16])), ALU.mult)
                it4 = itf.t[:].rearrange("p (h two) k -> p h two k", two=2)
                for w_, (op_, imm) in enumerate(((ALU.logical_shift_right, 4), (ALU.bitwise_and, 15))):
                    S.ts(cpi[:], cp[:], imm, op_)
                    S.copy(kf[w_][:], cpi.v(cpi.t[:].rearrange("p h k -> p (h k)")))
                    S.tt(oh[:], iota16, kf[w_].v(kf[w_].t[:].unsqueeze(2).to_broadcast([128, 128, 16])), ALU.is_equal)
                    S.tt(oh.v(oh.t[:].rearrange("p (h k) a -> p h k a", k=16)),
                         oh.v(oh.t[:].rearrange("p (h k) a -> p h k a", k=16)),
                         itf.v(it4[:, :, w_, :].unsqueeze(2).to_broadcast([128, 8, 16, 16])), ALU.mult)
                    S.red(sel[w_][:], oh[:], ALU.add)
                for w_, src in enumerate((sel[0][:], sel[1][:], gg.v(gg.t[:].rearrange("p h k -> p (h k)")))):
                    S.tr(bk[w_][:, 0:128], src, self.ident_f[:])
                    S.copy(selT[w_][:, tsl], bk[w_][:, 0:128], eng="act")

        skew(S, list(range(ns)), [front, back])
        self.release(mkA)
        TG = 256
        G_all = self.sb("G_all", [128, TG, 128], BF16)
        CH = 16
        P1c = [self.sb("P1c%d" % i, [128, CH, 128], BF16) for i in range(2)]
        P2c = [self.sb("P2c%d" % i, [128, CH, 128], BF16) for i in range(2)]
        eqc = [self.sb("eqc%d" % i, [128, CH, 128], BF16) for i in range(2)]
        ut = [self.sb("ut%d" % i, [128, 8, 128], BF16) for i in range(5)]
        vb = [self.sb("vb%d" % i, [128, D], BF16) for i in range(5)]
        a_sb = [self.sb("a_sb%d" % i, [128, TG], F32) for i in range(2)]
        h_bf = [self.sb("h_bf%d" % i, [128, TG], BF16) for i in range(2)]
        gF = self.sb("gF", [128, D], F32)
        S.dma(gF[:], V(self.final_norm_g.t.partition_broadcast(128), "final_norm_g"), eng="sp")
        h2 = [self.sb("h2_%d" % i, [128, D], F32) for i in range(1)]
        junk = self.sb("junkp", [128, D], BF16)
        ss = self.sb("ssp", [128, 1], F32)
        rstd = self.sb("rstdp", [128, 1], F32)
        iota3 = self.iota_free.v(self.iota_free.t[:].unsqueeze(1).to_broadcast([128, CH, 128]))
        u3 = self.peer_u.t.rearrange("(i e) d -> i e d", e=128)
        v3 = self.peer_v.t.rearrange("(i e) d -> i e d", e=128)
        for ps_ in range(ntok // TG):
            t0 = ps_ * TG
            ng = 0
            for c0 in range(0, TG, CH):
                tcs = slice(t0 + c0, t0 + c0 + CH)
                bc = lambda w_: selT[w_].v(selT[w_].t[:, tcs].unsqueeze(2).to_broadcast([128, CH, 128]))
                cpar = (c0 // CH) % 2
                S.tt(P2c[cpar][:], iota3, bc(1), ALU.is_equal)
                S.tt(eqc[cpar][:], iota3, bc(0), ALU.is_equal)
                S.tt(P1c[cpar][:], eqc[cpar][:], bc(2), ALU.mult)
                for k4 in range(CH // 4):
                    pb = bk[4 + ng % 2]
                    ng += 1
                    for k in range(4):
                        tk = k4 * 4 + k
                        S.mm(pb[:, k * 128:(k + 1) * 128], P2c[cpar][:, tk, :], P1c[cpar][:, tk, :])
                    S.copy(G_all[:, c0 + k4 * 4:c0 + k4 * 4 + 4, :],
                           pb.v(pb.t[:].rearrange("p (k i) -> p k i", i=128)), eng="act")
            def stL0(i, n):
                b = n % 5
                S.dma(ut[b][:], V(self.UT.t[i], "UT"), eng="sp")
                S.dma(vb[b][:], self.VB[i * 128:(i + 1) * 128, :], eng="act")

            def stA(i, n):
                b = n % 2
                pa = bk[4 + n % 2]
                for c in range(8):
                    S.mm(pa[:, 0:TG], ut[n % 5][:, c, :], self.xn2T[:, c, t0:t0 + TG], start=(c == 0), stop=(c == 7))
                S.act(a_sb[b][:], pa[:, 0:TG], AF.Gelu)
                S.tt(h_bf[b][:], a_sb[b][:], G_all[:, :, i], ALU.mult)

            def stV(i, n):
                b = n % 2
                for s_ in range(TG // 128):
                    for hf in range(2):
                        S.mm(bk[2 * s_ + hf][:], h_bf[b][:, s_ * 128:(s_ + 1) * 128], vb[n % 5][:, hf * 512:(hf + 1) * 512],
                             start=(n == 0), stop=(n == nexp - 1))

            skew(S, list(range(nexp)), [stL0, None, None, stA, stV])
            for s_ in range(TG // 128):
                m = ps_ * (TG // 128) + s_
                b = 0
                S.dma(h2[b][:], self.H1[m * 128:(m + 1) * 128, :], eng="sp")
                for hf in range(2):
                    S.tt(h2[b][:, hf * 512:(hf + 1) * 512], h2[b][:, hf * 512:(hf + 1) * 512], bk[2 * s_ + hf][:], ALU.add)
                S.act(junk[:], h2[b][:], AF.Square, accum=ss[:])
                S.act(rstd[:], ss[:], AF.Ln, bias=self.eps_t[:], scale=1.0 / D)
                S.act(rstd[:], rstd[:], AF.Exp, scale=-0.5)
                S.stt(h2[b][:], h2[b][:], rstd[:], gF[:], ALU.mult, ALU.mult)
                S.dma(self.out[m * 128:(m + 1) * 128, :], h2[b][:], eng="act")

    def sb_head(self, kt_, va_, qt_, hh, sbm, e_sb, sp_bf, t1, w_bf, R_sb, osb, pz, pC, pR, pO):
        S = self.S
        items = [(m, g) for m in range(self.ns) for g in range(m, -1, -1)]

        def stA(it, i):
            m, g = it
            k = i % 2
            qv = V(qt_.t[:, m * 128:(m + 1) * 128], "%s/m%d" % (qt_.name, m))
            for a in range(4):
                kt = 4 * g + a
                S.mm(pz[i % 3][:, a * 128:(a + 1) * 128], kt_[:, kt * 128:(kt + 1) * 128], qv)
            S.act(e_sb[k][:], pz[i % 3][:], AF.Exp, scale=0.125)
            S.act(sp_bf[k][:], e_sb[k][:], AF.Ln, bias=1.0, scale=1.0)
            if g == m:
                S.tt(sp_bf[k][:], sp_bf[k][:], sbm[:], ALU.mult)

        def stB(it, i):
            m, g = it
            k = i % 2
            S.mm(pC[k][:], self.tri_bf[:], sp_bf[k][:], start=True, stop=False)
            for s_ in range(1, 4):
                S.mm(pC[k][:, 0:(4 - s_) * 128], self.ones_bf[:], sp_bf[k][:, s_ * 128:512],
                     start=False, stop=(s_ == 3))
            for a in range(4):
                S.mm(pR[:, 0:128], self.ones_bf[:], sp_bf[k][:, a * 128:(a + 1) * 128],
                     start=(a == 0), stop=(a == 3))
            if g == m:
                S.copy(t1[k][:], pC[k][:])
                S.stt(t1[k][:], pz[i % 3][:], 0.125, t1[k][:], ALU.mult, ALU.subtract)
                S.copy(R_sb[:], pR[:, 0:128])
            else:
                S.tt(t1[k].v(t1[k].t[:].rearrange("p (a q) -> p a q", q=128)),
                     pC[k].v(pC[k].t[:].rearrange("p (a q) -> p a q", q=128)),
                     R_sb.v(R_sb.t[:].unsqueeze(1).to_broadcast([128, 4, 128])), ALU.add)
                S.stt(t1[k][:], pz[i % 3][:], 0.125, t1[k][:], ALU.mult, ALU.subtract)
                S.tt(R_sb[:], R_sb[:], pR[:, 0:128], ALU.add)

        def stB2(it, i):
            m, g = it
            k = i % 2
            S.act(w_bf[k][:], t1[k][:], AF.Exp)
            if g == m:
                S.tt(w_bf[k][:], w_bf[k][:], sbm[:], ALU.mult)

        def stC(it, i):
            m, g = it
            k = i % 2
            po = pO[m % 2]
            for a in range(4):
                kt = 4 * g + a
                S.mm(po[:, 0:DH], w_bf[k][:, a * 128:(a + 1) * 128], va_[:, kt, 0:DH],
                     start=(g == m and a == 0), stop=(g == 0 and a == 3))
            if g == 0:
                S.copy(osb[:, m, hh * DH:(hh + 1) * DH], po[:, 0:DH], eng="dve")

        skew(S, items, [stA, stB, stB2, stC])

    def moba_head(self, mh, kt_, va_, qt_, hh, osb, pz, pO, t1, w_bf, pTr):
        S = self.S
        h = mh
        slope = 2.0 ** (-(h + 1))
        cv = self.cv
        S.dma(self.kmf[:], V(self.KM.t[(h % 2) * DH:(h % 2 + 1) * DH, h // 2, :], "KM"), eng="sp")
        S.memset(self.kmb[:], 0.0)
        S.copy(self.kmb[0:DH, 0:32], self.kmf[:])
        pbs = self.banks[4]

        def qsub(m):
            return "%s/m%d" % (qt_.name, m)

        def gate1(m):
            nbc = 2 * m + 1
            S.mm(pbs[:, 0:nbc], V(qt_.t[:, m * 128:(m + 1) * 128], qsub(m)), self.kmb[:, 0:nbc])
            S.memset(self.bsm[:], -1e30)
            S.copy(self.bsm[:, 0:nbc], pbs[:, 0:nbc])
            S.ts(self.bsm[:, 2 * m:2 * m + 1], self.bsm[:, 2 * m:2 * m + 1], cv[:, 0:1], ALU.add)
            S.op("dve", lambda e, w=max(8, nbc): e.max(self.v8.t[:], self.bsm.t[:, 0:w]),
                 reads=[self.bsm[:]], writes=[self.v8[:]])
            S.ts(self.thr[:], self.v8[:, 2:3], -1e29, ALU.max)
            S.memset(self.selb[:], 0.0)
            S.ts(self.selb[:, 0:nbc], self.bsm[:, 0:nbc], self.thr[:], ALU.is_lt, NEG, ALU.mult)
            S.ts(self.selb[:, 2 * m:2 * m + 1], self.selb[:, 2 * m:2 * m + 1], cv[:, 1:2], ALU.mult)
            nb = 2 * m + 2
            S.ts(self.offm[:, 0:nb], self.iota_free[:, 0:nb], float(-2 * m), ALU.add, 2048.0 * slope, ALU.mult)
            S.memset(self.MBt2[m % 2][:], 0.0)
            S.tt(self.MBt2[m % 2][:, 68:68 + nb], self.offm[:, 0:nb], self.selb[:, 0:nb], ALU.add)
            S.memset(self.MBt2[m % 2][:, 64:65], 8.0 * slope)
            S.memset(self.MBt2[m % 2][:, 65:66], 1024.0 * slope)
            S.ts(self.MBt2[m % 2][:, 66:67], self.iota_part[:], -8.0 * slope, ALU.mult)
            S.copy(self.MBt2[m % 2][:, 67:68], cv[:, 8 + h:9 + h])

        def gate2(m):
            S.tr(pTr, self.MBt2[m % 2][:], self.ident_bf[:])
            S.copy(V(qt_.t[64:128, m * 128:(m + 1) * 128], qsub(m)),
                   V(self.banks_bf[4].t[64:128, 512:640], "bank4"), eng="act")

        gate1(0)
        gate2(0)
        if self.ns > 1:
            gate1(1)
            gate2(1)
        items = [(m, g) for m in range(self.ns) for g in range(m, -1, -1)]

        def stA(it, i):
            m, g = it
            k = i % 2
            k3 = i % 3
            qv = V(qt_.t[:, m * 128:(m + 1) * 128], qsub(m))
            for a in range(4):
                kt = 4 * g + a
                S.mm(pz[k3][:, a * 128:(a + 1) * 128], kt_[:, kt * 128:(kt + 1) * 128], qv)
            if g == m:
                S.tt(t1[k][:], pz[k3][:], self.mbn[:], ALU.add)
                S.act(w_bf[k3][:], t1[k][:], AF.Exp, scale=0.125)
            else:
                S.act(w_bf[k3][:], pz[k3][:], AF.Exp, scale=0.125)
            if g == m and m + 2 < self.ns:
                gate1(m + 2)
            if g == 0 and m + 2 < self.ns:
                gate2(m + 2)

        def stB(it, i):
            m, g = it
            k3 = i % 3
            po = pO[m % 2]
            for a in range(4):
                kt = 4 * g + a
                S.mm(po[:, 0:DH + 1], w_bf[k3][:, a * 128:(a + 1) * 128], va_[:, kt, :],
                     start=(g == m and a == 0), stop=(g == 0 and a == 3))
            if g == 0:
                S.copy(self.den[:], po[:, DH:DH + 1])
                S.recip(self.den[:], self.den[:])
                S.ts(osb[:, m, hh * DH:(hh + 1) * DH], po[:, 0:DH], self.den[:], ALU.mult)

        skew(S, items, [stA, None, stB])


def core_consts(j):
    kl = np.arange(128)[:, None]
    ql = np.arange(128)[None, :]
    sbm = np.zeros((128, 4, 128), np.float32)
    mbn = np.full((128, 4, 128), NEG, np.float32)
    for i in range(4):
        if i < j:
            sbm[:, i, :] = 1.0
        elif i == j:
            sbm[:, i, :] = (kl < ql)
    own = (0, 1) if j < 2 else (2, 3)
    for i in range(4):
        if j >= 2 and i < 2:
            mbn[:, i, :] = 0.0
        elif i in own:
            if i < j:
                mbn[:, i, :] = 0.0
            elif i == j:
                mbn[:, i, :] = np.where(kl <= ql, 0.0, NEG)
    kc = np.zeros((128, NT_ALL, 128), np.float32)
    kc[64] = np.arange(128)[None, :]
    for kt in range(NT_ALL):
        kc[65, kt, :] = kt % 2
        kc[66, kt, :] = 1.0
        kc[67, kt, :] = 1.0
        kc[68 + kt // 2, kt, :] = 1.0
    cvec = np.zeros((128, 16), np.float32)
    cvec[:, 0] = -1e30 if j < 2 else 0.0
    cvec[:, 1] = 0.0 if j < 2 else 1.0
    for h in range(8):
        cvec[:, 8 + h] = -1024.0 * (2.0 ** (-(h + 1))) * j
    return {"sbm01": sbm.reshape(128, 512), "mbneg": mbn.reshape(128, 512), "cvec": cvec,
            "kcst": kc.reshape(128, S_LEN)}


def host_inputs(inputs):
    x = np.asarray(inputs["x"], np.float32)
    maps = []
    for c in range(8):
        b, j = c // 4, c % 4
        xall = np.ascontiguousarray(x[b])
        tiles = xall.reshape(NT_ALL, 128, D)
        xown = np.ascontiguousarray(tiles[[4 * m + j for m in range(NT_OWN)]].reshape(NT_OWN * 128, D))
        m = {
            "xall": xall,
            "xown": xown,
            "norm1_g": np.ascontiguousarray(np.asarray(inputs["norm1_g"], np.float32)[0]),
            "w_in": np.ascontiguousarray(np.asarray(inputs["w_in"], np.float32)[0]),
        }
        for k in ("w_out_moba", "w_out_sb", "w_mix_out", "norm2_g", "peer_w_q", "peer_u", "peer_v"):
            m[k] = np.ascontiguousarray(np.asarray(inputs[k], np.float32)[0])
        m["peer_sub_keys"] = np.ascontiguousarray(np.asarray(inputs["peer_sub_keys"], np.float32)[0].reshape(16, 128, 128))
        m["final_norm_g"] = np.ascontiguousarray(np.asarray(inputs["final_norm_g"], np.float32))
        m.update(core_consts(j))
        maps.append(m)
    return maps


_CACHE = {}


def kernel(**inputs):
    if "nc" not in _CACHE:
        _CACHE["nc"] = Builder().build()
    nc = _CACHE["nc"]
    maps = host_inputs(inputs)
    res = run_bass_kernel_spmd(nc, maps, core_ids=list(range(8)))
    out = np.zeros((2, S_LEN, D), np.float32)
    for c in range(8):
        b, j = c // 4, c % 4
        o = np.asarray(res.results[c]["out"]).reshape(NT_OWN, 128, D)
        ov = out[b].reshape(NT_ALL, 128, D)
        for m in range(NT_OWN):
            ov[4 * m + j] = o[m]
    return out
```
